# Optimizing a Trainium2 kernel written in Bass

```python
import math
import jax
import jax.numpy as jnp
from jax import lax
import numpy as np

D_MODEL = 2048
BATCH = 8
SEQ = 2048
DEPTH = 2

GRID_W = 64
CTX_LEN = 256
N_MIXERS = 2
N_LAYERS_A = (DEPTH + N_MIXERS - 1) // N_MIXERS
N_LAYERS_B = DEPTH // N_MIXERS
NORM_EPS = 1e-6
N_MOD = 6

GMLP_CHUNK = 128
GMLP_WIDTH = D_MODEL
GMLP_GROUPS = 8

DIFF_HEADS = 8
DIFF_HEAD_DIM = D_MODEL // DIFF_HEADS // 2
QUERY_BLOCK = 128
ROPE_THETA = 10000.0

N_EXPERTS = 32
TOP_K = 4
D_EXPERT = D_MODEL
SWIGLU_ALPHA = 1.702
SWIGLU_LIMIT = 7.0
EXPERT_BLOCK = 128

kernel_name = "hybrid_gmlp_diffattn_moe_dit"


def rms_norm(x, g):
    xf = x.astype(jnp.float32)
    y = xf * lax.rsqrt(jnp.mean(xf * xf, axis=-1, keepdims=True) + NORM_EPS)
    return (y * g.astype(jnp.float32)).astype(x.dtype)


def layer_norm(x, g):
    xf = x.astype(jnp.float32)
    xc = xf - jnp.mean(xf, axis=-1, keepdims=True)
    y = xc * lax.rsqrt(jnp.mean(xc * xc, axis=-1, keepdims=True) + NORM_EPS)
    return (y * g.astype(jnp.float32)).astype(x.dtype)


def modulate(h, shift, scale):
    return h * (1 + scale) + shift


def axial_rope_tables(rows, dtype):
    row_pos = jnp.repeat(jnp.arange(rows), GRID_W).astype(jnp.float32)
    col_pos = jnp.tile(jnp.arange(GRID_W), rows).astype(jnp.float32)
    n_freq = DIFF_HEAD_DIM // 4
    inv_freq = ROPE_THETA ** (-jnp.arange(n_freq, dtype=jnp.float32) / n_freq)
    ang = jnp.stack([row_pos[:, None] * inv_freq, col_pos[:, None] * inv_freq], axis=1)
    return jnp.cos(ang)[:, None].astype(dtype), jnp.sin(ang)[:, None].astype(dtype)


def apply_axial_rope(x, cos, sin):
    b, l, h, dh = x.shape
    xr = x.reshape(b, l, h, 2, 2, dh // 4)
    x1, x2 = xr[..., 0, :], xr[..., 1, :]
    out = jnp.stack([x1 * cos - x2 * sin, x2 * cos + x1 * sin], axis=-2)
    return out.reshape(b, l, h, dh)


def chunk_gmlp(h, w_in, norm_g, w_s, b_s, w_out):
    b, l, _ = h.shape
    u, v = jnp.split(jax.nn.gelu(h @ w_in, approximate=False), 2, axis=-1)
    v = layer_norm(v, norm_g).reshape(b, l // GMLP_CHUNK, GMLP_CHUNK, GMLP_GROUPS, GMLP_WIDTH // GMLP_GROUPS)
    mixed = jnp.einsum('gpq,bnqgc->bnpgc', w_s, v) + b_s.T[:, :, None]
    return (u * mixed.reshape(b, l, GMLP_WIDTH)) @ w_out


def diff_attn_block(q, k, v, lam):
    s = jnp.einsum('bqhd,bkhd->bhqk', q, k).astype(jnp.float32) * (DIFF_HEAD_DIM ** -0.5)
    p = jax.nn.softmax(s, axis=-1)
    b, _, nq, nk = p.shape
    p = p.reshape(b, DIFF_HEADS, 2, nq, nk)
    a = p[:, :, 0] - lam * p[:, :, 1]
    return jnp.einsum('bhqk,bkhe->bqhe', a.astype(v.dtype), v)


def diff_attention(h_lat, h_ctx, cos, sin, layer_idx, need_ctx_out,
                   w_qkv, q_norm_g, k_norm_g, lam_vecs, subln_g, w_o):
    b, s_len, _ = h_lat.shape

    def project(h):
        l = h.shape[1]
        q, k, v = jnp.split(h @ w_qkv, 3, axis=-1)
        q = rms_norm(q.reshape(b, l, 2 * DIFF_HEADS, DIFF_HEAD_DIM), q_norm_g)
        k = rms_norm(k.reshape(b, l, 2 * DIFF_HEADS, DIFF_HEAD_DIM), k_norm_g)
        v = v.reshape(b, l, DIFF_HEADS, 2 * DIFF_HEAD_DIM)
        return q, k, v

    q_lat, k_lat, v_lat = project(h_lat)
    q_ctx, k_ctx, v_ctx = project(h_ctx)
    q_lat = apply_axial_rope(q_lat, cos, sin)
    k_lat = apply_axial_rope(k_lat, cos, sin)

    lam_init = 0.8 - 0.6 * math.exp(-0.3 * layer_idx)
    lf = lam_vecs.astype(jnp.float32)
    lam = jnp.exp(jnp.sum(lf[0] * lf[1])) - jnp.exp(jnp.sum(lf[2] * lf[3])) + lam_init

    k_all = jnp.concatenate([k_ctx, k_lat], axis=1)
    v_all = jnp.concatenate([v_ctx, v_lat], axis=1)
    n_blk = s_len // QUERY_BLOCK
    q_blocks = q_lat.reshape(b, n_blk, QUERY_BLOCK, 2 * DIFF_HEADS, DIFF_HEAD_DIM).transpose(1, 0, 2, 3, 4)
    o = lax.map(lambda qb: diff_attn_block(qb, k_all, v_all, lam), q_blocks)
    o_lat = o.transpose(1, 0, 2, 3, 4).reshape(b, s_len, DIFF_HEADS, 2 * DIFF_HEAD_DIM)

    def finish(o_heads):
        o_heads = rms_norm(o_heads, subln_g) * (1 - lam_init)
        return o_heads.reshape(b, o_heads.shape[1], D_MODEL) @ w_o

    y_lat = finish(o_lat)
    y_ctx = finish(diff_attn_block(q_ctx, k_ctx, v_ctx, lam)) if need_ctx_out else None
    return y_lat, y_ctx


def moe_ffn(h, w_router, b_router, w_gu, b_gu, w_down, b_down):
    n_tok, d = h.shape
    logits = (h @ w_router + b_router).astype(jnp.float32)
    top_logit, top_idx = lax.top_k(logits, TOP_K)
    top_w = jax.nn.softmax(top_logit, axis=-1).astype(h.dtype)

    n_slots = n_tok * TOP_K
    slot_e = top_idx.reshape(-1)
    slot_tok = jnp.repeat(jnp.arange(n_tok, dtype=jnp.int32), TOP_K)
    slot_w = top_w.reshape(-1)
    order = jnp.argsort(slot_e)
    e_sorted, tok_sorted, w_sorted = slot_e[order], slot_tok[order], slot_w[order]

    counts = jnp.bincount(slot_e, length=N_EXPERTS)
    starts = jnp.cumsum(counts) - counts
    padded = (counts + EXPERT_BLOCK - 1) // EXPERT_BLOCK * EXPERT_BLOCK
    pad_ends = jnp.cumsum(padded)
    pad_starts = pad_ends - padded
    dest = pad_starts[e_sorted] + jnp.arange(n_slots, dtype=jnp.int32) - starts[e_sorted]

    n_blocks = -(-(n_slots + N_EXPERTS * (EXPERT_BLOCK - 1)) // EXPERT_BLOCK)
    n_pad = n_blocks * EXPERT_BLOCK
    buf_tok = jnp.full((n_pad,), n_tok, jnp.int32).at[dest].set(tok_sorted)
    buf_w = jnp.zeros((n_pad,), h.dtype).at[dest].set(w_sorted)
    blk_e = jnp.minimum(jnp.searchsorted(pad_ends, jnp.arange(n_blocks) * EXPERT_BLOCK, side='right'),
                        N_EXPERTS - 1)
    h_pad = jnp.concatenate([h, jnp.zeros((1, d), h.dtype)], axis=0)

    def expert_block(args):
        tok, e = args
        gu = h_pad[tok] @ w_gu[e] + b_gu[e]
        glu = jnp.minimum(gu[:, 0::2], SWIGLU_LIMIT)
        lin = jnp.clip(gu[:, 1::2], -SWIGLU_LIMIT, SWIGLU_LIMIT)
        act = glu * jax.nn.sigmoid(SWIGLU_ALPHA * glu) * (lin + 1)
        return act @ w_down[e] + b_down[e]

    y = lax.map(expert_block, (buf_tok.reshape(n_blocks, EXPERT_BLOCK), blk_e))
    y = y.reshape(n_pad, d) * buf_w[:, None]
    return jax.ops.segment_sum(y, buf_tok, num_segments=n_tok + 1)[:n_tok]


def setup_inputs(seed: int = 0) -> dict:
    key = jax.random.key(seed)
    ks = jax.random.split(key, 26)
    f32 = jnp.float32
    D = D_MODEL
    HD = DIFF_HEAD_DIM

    def nrm(k, shape, s):
        return jax.random.normal(k, shape, f32) * s

    return {
        "x": nrm(ks[0], (BATCH, SEQ, D), 1.0),
        "c": nrm(ks[1], (BATCH, D), 1.0),
        "ctx": nrm(ks[2], (BATCH, CTX_LEN, D), 1.0),
        "c_ctx": nrm(ks[3], (D,), 1.0),
        "w_mod": nrm(ks[4], (DEPTH, D, N_MOD * D), 0.5 * D ** -0.5),
        "b_mod": nrm(ks[5], (DEPTH, N_MOD * D), 0.01),
        "norm_mix_g": 1.0 + nrm(ks[6], (DEPTH, D), 0.05),
        "norm_ffn_g": 1.0 + nrm(ks[7], (DEPTH, D), 0.05),
        "gmlp_w_in": nrm(ks[8], (N_LAYERS_A, D, 2 * GMLP_WIDTH), D ** -0.5),
        "gmlp_norm_g": 1.0 + nrm(ks[9], (N_LAYERS_A, GMLP_WIDTH), 0.05),
        "gmlp_w_s": nrm(ks[10], (N_LAYERS_A, GMLP_GROUPS, GMLP_CHUNK, GMLP_CHUNK), GMLP_CHUNK ** -0.5),
        "gmlp_b_s": 1.0 + nrm(ks[11], (N_LAYERS_A, GMLP_GROUPS, GMLP_CHUNK), 0.05),
        "gmlp_w_out": nrm(ks[12], (N_LAYERS_A, GMLP_WIDTH, D), GMLP_WIDTH ** -0.5),
        "diff_w_qkv": nrm(ks[13], (N_LAYERS_B, D, 3 * D), D ** -0.5),
        "diff_q_norm_g": 1.0 + nrm(ks[14], (N_LAYERS_B, HD), 0.05),
        "diff_k_norm_g": 1.0 + nrm(ks[15], (N_LAYERS_B, HD), 0.05),
        "diff_lambda": nrm(ks[16], (N_LAYERS_B, 4, HD), 0.1),
        "diff_subln_g": 1.0 + nrm(ks[17], (N_LAYERS_B, 2 * HD), 0.05),
        "diff_w_o": nrm(ks[18], (N_LAYERS_B, D, D), D ** -0.5),
        "moe_w_router": nrm(ks[19], (DEPTH, D, N_EXPERTS), D ** -0.5),
        "moe_b_router": nrm(ks[20], (DEPTH, N_EXPERTS), 0.01),
        "moe_w_gate_up": nrm(ks[21], (DEPTH, N_EXPERTS, D, 2 * D_EXPERT), D ** -0.5),
        "moe_b_gate_up": nrm(ks[22], (DEPTH, N_EXPERTS, 2 * D_EXPERT), 0.01),
        "moe_w_down": nrm(ks[23], (DEPTH, N_EXPERTS, D_EXPERT, D), D_EXPERT ** -0.5),
        "moe_b_down": nrm(ks[24], (DEPTH, N_EXPERTS, D), 0.01),
    }


def reference(x, c, ctx, c_ctx, w_mod, b_mod, norm_mix_g, norm_ffn_g,
              gmlp_w_in, gmlp_norm_g, gmlp_w_s, gmlp_b_s, gmlp_w_out,
              diff_w_qkv, diff_q_norm_g, diff_k_norm_g, diff_lambda, diff_subln_g, diff_w_o,
              moe_w_router, moe_b_router, moe_w_gate_up, moe_b_gate_up, moe_w_down, moe_b_down):
    b, s_len, d = x.shape
    rows = s_len // GRID_W
    cos, sin = axial_rope_tables(rows, x.dtype)
    silu_c = jax.nn.silu(c)
    silu_cc = jax.nn.silu(c_ctx)
    x_lat, x_ctx = x, ctx

    for i in range(DEPTH):
        need_ctx = i < DEPTH - 1
        j = i // N_MIXERS
        mod_lat = (silu_c @ w_mod[i] + b_mod[i]).reshape(b, 1, N_MOD, d)
        mod_ctx = (silu_cc @ w_mod[i] + b_mod[i]).reshape(1, 1, N_MOD, d)

        h_lat = modulate(rms_norm(x_lat, norm_mix_g[i]), mod_lat[:, :, 0], mod_lat[:, :, 1])
        h_ctx = modulate(rms_norm(x_ctx, norm_mix_g[i]), mod_ctx[:, :, 0], mod_ctx[:, :, 1])
        if i % N_MIXERS == 0:
            gp = (gmlp_w_in[j], gmlp_norm_g[j], gmlp_w_s[j], gmlp_b_s[j], gmlp_w_out[j])
            y_lat = chunk_gmlp(h_lat, *gp)
            y_ctx = chunk_gmlp(h_ctx, *gp) if need_ctx else None
        else:
            y_lat, y_ctx = diff_attention(h_lat, h_ctx, cos, sin, i, need_ctx,
                                          diff_w_qkv[j], diff_q_norm_g[j], diff_k_norm_g[j],
                                          diff_lambda[j], diff_subln_g[j], diff_w_o[j])
        x_lat = x_lat + mod_lat[:, :, 2] * y_lat

        mp = (moe_w_router[i], moe_b_router[i], moe_w_gate_up[i], moe_b_gate_up[i],
              moe_w_down[i], moe_b_down[i])
        h_lat = modulate(rms_norm(x_lat, norm_ffn_g[i]), mod_lat[:, :, 3], mod_lat[:, :, 4])
        if need_ctx:
            x_ctx = x_ctx + mod_ctx[:, :, 2] * y_ctx
            h_ctx = modulate(rms_norm(x_ctx, norm_ffn_g[i]), mod_ctx[:, :, 3], mod_ctx[:, :, 4])
            n_lat = b * s_len
            y_all = moe_ffn(jnp.concatenate([h_lat.reshape(-1, d), h_ctx.reshape(-1, d)], axis=0), *mp)
            x_ctx = x_ctx + mod_ctx[:, :, 5] * y_all[n_lat:].reshape(b, -1, d)
            y_lat = y_all[:n_lat].reshape(b, s_len, d)
        else:
            y_lat = moe_ffn(h_lat.reshape(-1, d), *mp).reshape(b, s_len, d)
        x_lat = x_lat + mod_lat[:, :, 5] * y_lat

    return x_lat
```

```python
import contextlib
import numpy as np
import ml_dtypes
import concourse.bass as bass
import concourse.mybir as mybir
from concourse.bass_utils import run_bass_kernel_spmd

F32 = mybir.dt.float32
BF16 = mybir.dt.bfloat16
ALU = mybir.AluOpType
ACTF = mybir.ActivationFunctionType
AX = mybir.AxisListType

D = 2048
KC = 16
NCORE = 8
SEQ = 2048
CTX = 256
NTOK = SEQ + CTX
NTILE = NTOK // 128
NEXP = 32
EPC = 4
EPS = 1e-6
HD = 128
NH2 = 16
NH = 8
ALPHA = 1.702
LIMIT = 7.0
GELU_FUNC = [ACTF.Gelu]


class Sched:
    def __init__(self, nc, stack, n_dma=10):
        self.nc = nc
        self.eng = {"pe": nc.tensor, "act": nc.scalar, "dve": nc.vector, "pool": nc.gpsimd, "sp": nc.sync}
        self.sem = {}
        self.cnt = {}
        self.known = {k: {} for k in self.eng}
        for k in self.eng:
            self.sem[k] = stack.enter_context(nc.semaphore("s_" + k))
            self.cnt[k] = 0
        self.dma_sems = {}
        self.dma_rr = {}
        for q in ("sp", "pool", "act"):
            self.dma_sems[q] = [[stack.enter_context(nc.semaphore(f"d_{q}{i}")), f"d_{q}{i}", 0] for i in range(n_dma)]
            self.dma_rr[q] = 0
        self.last_w = {}
        self.readers = {}

    def _wait(self, e, tok):
        sem, key, val, owner = tok
        if self.known[e].get(key, 0) >= val:
            return
        self.eng[e].wait_ge(sem, val)
        self.known[e][key] = val

    def _deps(self, e, reads, writes):
        for b in reads:
            for t in self.last_w.get(b, {}).values():
                self._wait(e, t)
        for b in writes:
            for t in self.last_w.get(b, {}).values():
                if t[3] != e:
                    self._wait(e, t)
            for t in self.readers.get(b, {}).values():
                if t[3] != e:
                    self._wait(e, t)

    def _record(self, tok, reads, writes):
        for b in reads:
            self.readers.setdefault(b, {})[tok[1]] = tok
        for b in writes:
            prev = self.last_w.get(b, {})
            if tok[3] == "dma" and prev and all(t[3] == "dma" for t in prev.values()) and not self.readers.get(b):
                prev[tok[1]] = tok
            else:
                self.last_w[b] = {tok[1]: tok}
            self.readers[b] = {}

    def op(self, e, fn, reads=(), writes=()):
        self._deps(e, reads, writes)
        ins = fn(self.eng[e])
        self.cnt[e] += 1
        ins.then_inc(self.sem[e], 1)
        tok = (self.sem[e], e, self.cnt[e], e)
        self._record(tok, reads, writes)
        return tok

    def group(self, e, fns, reads=(), writes=()):
        self._deps(e, reads, writes)
        ins = None
        for fn in fns:
            ins = fn(self.eng[e])
        self.cnt[e] += 1
        ins.then_inc(self.sem[e], 1)
        tok = (self.sem[e], e, self.cnt[e], e)
        self._record(tok, reads, writes)
        return tok

    def dma(self, q, out, in_, reads=(), writes=(), **kw):
        self._deps(q, reads, writes)
        lst = self.dma_sems[q]
        ent = lst[self.dma_rr[q] % len(lst)]
        self.dma_rr[q] += 1
        if ent[2] > 0:
            self._wait(q, (ent[0], ent[1], ent[2], "dma"))
        ins = self.eng[q].dma_start(out=out, in_=in_, **kw)
        ent[2] += 16
        ins.then_inc(ent[0], 16)
        tok = (ent[0], ent[1], ent[2], "dma")
        self._record(tok, reads, writes)
        return tok

    def barrier(self):
        toks = [(self.sem[k], k, self.cnt[k], k) for k in self.eng if self.cnt[k] > 0]
        for q in self.dma_sems:
            for ent in self.dma_sems[q]:
                if ent[2] > 0:
                    toks.append((ent[0], ent[1], ent[2], "dma"))
        for e in self.eng:
            for t in toks:
                if t[3] != e:
                    self._wait(e, t)

    def finish(self):
        for q in self.dma_sems:
            for ent in self.dma_sems[q]:
                if ent[2] > 0:
                    self._wait("sp", (ent[0], ent[1], ent[2], "dma"))


_UQ = [0]


def _uq(n):
    _UQ[0] += 1
    return f"{n}_{_UQ[0]}"


def _new_nc():
    _UQ[0] = 0
    return bass.Bass("TRN2", target_bir_lowering=False)


def _bcast(ap, shape):
    return ap.to_broadcast(list(shape))


MCOLS = 6 * D // NCORE


def build_mod():
    nc = _new_nc()
    cT = nc.dram_tensor("cT", [128, KC, 9], F32, kind="ExternalInput").ap()
    wm = nc.dram_tensor("wm", [2, 128, KC, MCOLS], F32, kind="ExternalInput").ap()
    bm = nc.dram_tensor("bm", [2, MCOLS], F32, kind="ExternalInput").ap()
    out = nc.dram_tensor("mod", [2, 9, MCOLS], F32, kind="ExternalOutput").ap()
    with contextlib.ExitStack() as st:
        S = Sched(nc, st)
        sb = lambda n, sh, dt: st.enter_context(nc.sbuf_tensor(_uq("sb_" + n), sh, dt))
        cs = sb("cs", [128, KC, 9], F32)
        css = sb("css", [128, KC, 9], F32)
        wt = [sb(f"wt{i}", [128, KC, 512], F32) for i in range(2)]
        bt = sb("bt", [9, 2, MCOLS], F32)
        ot = [sb(f"ot{i}", [9, 512], F32) for i in range(2)]
        ps = [st.enter_context(nc.psum_tensor(f"ps{i}", [9, 512], F32)) for i in range(2)]
        S.dma("sp", cs[:], cT, writes=["cs"])
        for l in range(2):
            S.dma("sp", bt[:, l, :], bm[l:l + 1, :].partition_broadcast(9) if False else bm[l:l + 1, :].to_broadcast([9, MCOLS]),
                  writes=["bt"])
        S.op("act", lambda e: e.activation(out=css[:], in_=cs[:], func=ACTF.Silu), reads=["cs"], writes=["css"])
        i = 0
        for l in range(2):
            for g in range(MCOLS // 512):
                w = wt[i % 2]
                S.dma("sp", w[:], wm[l, :, :, g * 512:(g + 1) * 512], writes=[f"wt{i % 2}"])
                p = ps[i % 2]
                S.group("pe", [
                    (lambda e, k=k, w=w, p=p: e.matmul(p[:], css[:, k, :], w[:, k, :], start=(k == 0), stop=(k == KC - 1)))
                    for k in range(KC)], reads=["css", f"wt{i % 2}"], writes=[f"ps{i % 2}"])
                o = ot[i % 2]
                S.op("dve", lambda e, o=o, p=p, l=l, g=g: e.tensor_tensor(out=o[:], in0=p[:], in1=bt[:, l, g * 512:(g + 1) * 512], op=ALU.add),
                     reads=[f"ps{i % 2}", "bt"], writes=[f"ot{i % 2}"])
                S.dma("sp", out[l, :, g * 512:(g + 1) * 512], o[:], reads=[f"ot{i % 2}"], writes=["out"])
                i += 1
        S.finish()
    return nc


def run_mod(c, c_ctx, w_mod, b_mod):
    c_all = np.concatenate([c, c_ctx[None, :]], axis=0)
    cT = np.ascontiguousarray(c_all.T.reshape(KC, 128, 9).transpose(1, 0, 2))
    in_maps = []
    for core in range(NCORE):
        sl = slice(core * MCOLS, (core + 1) * MCOLS)
        wm = np.ascontiguousarray(w_mod[:, :, sl].reshape(2, KC, 128, MCOLS).transpose(0, 2, 1, 3))
        in_maps.append({"cT": cT, "wm": wm, "bm": np.ascontiguousarray(b_mod[:, sl])})
    nc = build_mod()
    res = run_bass_kernel_spmd(nc, in_maps, core_ids=list(range(NCORE)))
    mod = np.concatenate([r["mod"] for r in res.results], axis=2)
    return mod.reshape(2, 9, 6, D)


def _b3(ap2, n):
    return ap2.unsqueeze(2).to_broadcast([ap2.shape[0], ap2.shape[1], n])


def emit_rstd(S, x_t, xk, sq, ss, rstd, n):
    S.op("act", lambda e: e.activation(out=sq, in_=x_t, func=ACTF.Square, accum_out=ss), reads=[xk], writes=["sq", "ss"])
    S.op("act", lambda e: e.activation(out=rstd, in_=ss, func=ACTF.Sqrt, scale=1.0 / n, bias=EPS), reads=["ss"], writes=["rstd"])
    S.op("dve", lambda e: e.reciprocal(out=rstd, in_=rstd), reads=["rstd"], writes=["rstd"])


def emit_ffn_prep(S, T, x1, x1k, A2, B2, tok0, htm_out, gates_out, var="l"):
    emit_rstd(S, x1[:], x1k, T["sq"][:], T["ss"][:], T["rstd"][:], D)
    S.op("dve", lambda e: e.tensor_scalar(out=T["xnf"][:], in0=x1[:], scalar1=T["rstd"][:, 0:1], scalar2=None, op0=ALU.mult),
         reads=[x1k, "rstd"], writes=["xnf"])
    pT = T["pT32"]
    S.group("pe", [(lambda e, k=k: e.transpose(out=pT[:, k, :], in_=T["xnf"][:, k * 128:(k + 1) * 128], identity=T["ident_f"][:]))
                   for k in range(KC)], reads=["xnf", "ident_f"], writes=["pT32"])
    h32 = T["h32"]
    S.op("dve", lambda e: e.tensor_tensor(out=h32[:], in0=pT[:], in1=_b3(A2, 128), op=ALU.mult), reads=["pT32", "vec"], writes=["h32"])
    S.op("dve", lambda e: e.tensor_tensor(out=h32[:], in0=h32[:], in1=_b3(B2, 128), op=ALU.add), reads=["h32", "vec"], writes=["h32"])
    S.op("dve", lambda e: e.tensor_tensor(out=T["sq"][:], in0=T["xnf"][:], in1=T["A2tm_" + var][:], op=ALU.mult), reads=["xnf", "A2tm"], writes=["sq"])
    S.op("dve", lambda e: e.tensor_tensor(out=T["htmb"][:], in0=T["sq"][:], in1=T["B2tm_" + var][:], op=ALU.add), reads=["sq", "A2tm"], writes=["htmb"])
    S.dma("sp", htm_out[tok0:tok0 + 128, :], T["htmb"][:], reads=["htmb"], writes=["htm_out"])
    pr = T["pR"]
    S.group("pe", [(lambda e, k=k: e.matmul(pr[:], h32[:, k, :], T["wr"][:, k, :], start=(k == 0), stop=(k == KC - 1)))
                   for k in range(KC)], reads=["h32", "wr"], writes=["pR"])
    lg, m8, ex, msk = T["lg"], T["m8"], T["ex"], T["msk"]
    S.op("dve", lambda e: e.tensor_tensor(out=lg[:], in0=pr[:], in1=T["br"][:], op=ALU.add), reads=["pR", "br"], writes=["lg"])
    S.op("dve", lambda e: e.max(out=m8[:], in_=lg[:]), reads=["lg"], writes=["m8"])
    S.op("dve", lambda e: e.tensor_scalar(out=msk[:], in0=lg[:], scalar1=m8[:, 3:4], scalar2=None, op0=ALU.is_ge), reads=["lg", "m8"], writes=["msk"])
    S.op("dve", lambda e: e.tensor_scalar(out=T["nmx"][:], in0=m8[:, 0:1], scalar1=-1.0, scalar2=None, op0=ALU.mult), reads=["m8"], writes=["nmx"])
    S.op("act", lambda e: e.activation(out=ex[:], in_=lg[:], func=ACTF.Exp, bias=T["nmx"][:, 0:1]), reads=["lg", "nmx"], writes=["ex"])
    S.op("dve", lambda e: e.tensor_tensor(out=ex[:], in0=ex[:], in1=msk[:], op=ALU.mult), reads=["ex", "msk"], writes=["ex"])
    S.op("dve", lambda e: e.reduce_sum(out=T["sm"][:], in_=ex[:], axis=AX.X), reads=["ex"], writes=["sm"])
    S.op("dve", lambda e: e.reciprocal(out=T["sm"][:], in_=T["sm"][:]), reads=["sm"], writes=["sm"])
    S.op("dve", lambda e: e.tensor_scalar(out=T["gt"][:], in0=ex[:], scalar1=T["sm"][:, 0:1], scalar2=None, op0=ALU.mult), reads=["ex", "sm"], writes=["gt"])
    S.dma("sp", gates_out[tok0:tok0 + 128, :], T["gt"][:], reads=["gt"], writes=["gates_out"])


def alloc_ffn_prep(nc, st, S, T, wr_d, br_d, identf_d, vtm_d, variants):
    sb = lambda n, sh, dt: st.enter_context(nc.sbuf_tensor(_uq("sb_" + n), sh, dt))
    for n, sh, dt in (("sq", [128, D], F32), ("ss", [128, 1], F32), ("rstd", [128, 1], F32), ("xnf", [128, D], F32),
                      ("h32", [128, KC, 128], F32), ("htmb", [128, D], BF16), ("wr", [128, KC, NEXP], F32),
                      ("br", [128, NEXP], F32), ("ident_f", [128, 128], F32), ("lg", [128, NEXP], F32), ("m8", [128, 8], F32),
                      ("ex", [128, NEXP], F32), ("msk", [128, NEXP], F32), ("nmx", [128, 1], F32), ("sm", [128, 1], F32),
                      ("gt", [128, NEXP], F32)):
        if n not in T:
            T[n] = sb(n, sh, dt)
    S.dma("sp", T["wr"][:], wr_d, writes=["wr"])
    S.dma("sp", T["br"][:], br_d.to_broadcast([128, NEXP]), writes=["br"])
    S.dma("sp", T["ident_f"][:], identf_d, writes=["ident_f"])
    for v, (gr, shr, scr) in variants.items():
        A = T["A2tm_" + v] = sb("A2tm_" + v, [128, D], F32)
        B = T["B2tm_" + v] = sb("B2tm_" + v, [128, D], F32)
        S.dma("sp", A[:], vtm_d[gr:gr + 1, :].to_broadcast([128, D]), writes=["A2tm"])
        S.dma("sp", T["xnf"][:], vtm_d[scr:scr + 1, :].to_broadcast([128, D]), writes=["xnf"])
        S.dma("sp", B[:], vtm_d[shr:shr + 1, :].to_broadcast([128, D]), writes=["A2tm"])
        S.op("dve", lambda e, A=A: e.scalar_tensor_tensor(out=A[:], in0=T["xnf"][:], scalar=1.0, in1=A[:], op0=ALU.add, op1=ALU.mult),
             reads=["xnf", "A2tm"], writes=["A2tm"])


V_GMIX, V_SH_L, V_SC_L, V_SH_C, V_SC_C, V_GFFN, V_SH2_L, V_SC2_L, V_SH2_C, V_SC2_C = range(10)
NVF = 10


def emit_modvecs(S, T, vfm):
    res = {}
    for nm, g, sh, sc in (("mix_l", V_GMIX, V_SH_L, V_SC_L), ("mix_c", V_GMIX, V_SH_C, V_SC_C),
                          ("ffn_l", V_GFFN, V_SH2_L, V_SC2_L), ("ffn_c", V_GFFN, V_SH2_C, V_SC2_C)):
        a = T["A_" + nm]
        S.op("dve", lambda e, a=a, g=g, sc=sc: e.scalar_tensor_tensor(out=a[:], in0=vfm[:, sc, :], scalar=1.0, in1=vfm[:, g, :],
                                                                    op0=ALU.add, op1=ALU.mult), reads=["vec"], writes=["vec"])
        res[nm] = (a[:], vfm[:, sh, :])
    return res


def build_gmlp(dbg=False):
    nc = _new_nc()
    din = lambda n, sh, dt=F32: nc.dram_tensor(n, sh, dt, kind="ExternalInput").ap()
    xa = din("xa", [NTOK, D])
    w_in = din("w_in", [128, KC, 2 * D])
    w_out = din("w_out", [128, KC, D])
    wsT_d = din("wsT", [128, 8, 128])
    vfm_d = din("vfm", [128, NVF, KC])
    vtm_d = din("vtm", [8, D])
    bs_d = din("bs", [128, 8])
    wr_d = din("wr", [128, KC, NEXP])
    br_d = din("br", [1, NEXP])
    identf_d = din("identf", [128, 128])
    identb_d = din("identb", [128, 128], BF16)
    x1_o = nc.dram_tensor("x1", [NTOK, D], F32, kind="ExternalOutput").ap()
    hT_o = nc.dram_tensor("htm", [NTOK, D], BF16, kind="ExternalOutput").ap()
    gates_o = nc.dram_tensor("gates", [NTOK, NEXP], F32, kind="ExternalOutput").ap()
    gts = nc.dram_tensor("gts", [NTILE, 128, KC, 128], BF16).ap()
    if dbg:
        dbo = {n: nc.dram_tensor("dbg_" + n, sh, dt, kind="ExternalOutput").ap() for n, sh, dt in (
            ("hT", [128, KC, 128], BF16), ("u", [128, D], F32), ("v", [128, D], F32), ("vn", [128, D], BF16),
            ("mix", [128, D], F32), ("gated", [128, D], BF16), ("xn", [128, D], BF16), ("w0", [128, KC, 128], BF16), ("w2", [128, KC, 128], BF16))}
    with contextlib.ExitStack() as st0:
        S = Sched(nc, st0)
        T = {}
        sb0 = lambda n, sh, dt: st0.enter_context(nc.sbuf_tensor(_uq("sb_" + n), sh, dt))
        vfm = sb0("vfm", [128, NVF, KC], F32)
        for nm in ("mix_l", "mix_c", "ffn_l", "ffn_c"):
            T["A_" + nm] = sb0("A_" + nm, [128, KC], F32)
        T["ident_b"] = sb0("ident_b", [128, 128], BF16)
        x_t = sb0("x_t", [128, D], F32)
        T["sq"] = sb0("sq", [128, D], F32)
        T["ss"] = sb0("ss", [128, 1], F32)
        T["rstd"] = sb0("rstd", [128, 1], F32)
        gT = sb0("gT", [128, KC, 128], BF16)
        S.dma("sp", vfm[:], vfm_d, writes=["vec"])
        S.dma("sp", T["ident_b"][:], identb_d, writes=["ident_b"])
        mv = emit_modvecs(S, T, vfm)
        with contextlib.ExitStack() as st:
            sb = lambda n, sh, dt: st.enter_context(nc.sbuf_tensor(_uq("sb_" + n), sh, dt))
            ps = lambda n, sh, dt: st.enter_context(nc.psum_tensor(_uq("ps_" + n), sh, dt))
            win = sb("win", [128, KC, 2 * D], BF16)
            wsT = sb("wsT_s", [128, 8, 128], BF16)
            bs = sb("bs_s", [128, 8], F32)
            lng = sb("lng", [128, D], F32)
            xn = sb("xn", [128, D], BF16)
            hT = sb("hT_s", [128, KC, 128], BF16)
            u = sb("u", [128, D], F32)
            v = sb("v", [128, D], F32)
            vn = sb("vn", [128, D], BF16)
            gated = sb("gated", [128, D], BF16)
            mean = sb("mean", [128, 1], F32)
            pT = ps("pT", [128, KC, 128], BF16)
            pU = [ps(f"pU{i}", [128, 512], F32) for i in range(2)]
            pS = ps("pS", [128, D], F32)
            for c4 in range(4):
                S.dma("pool", win[:, :, c4 * 1024:(c4 + 1) * 1024], w_in[:, :, c4 * 1024:(c4 + 1) * 1024], writes=["win"])
            S.dma("pool", wsT[:], wsT_d, writes=["wsT"])
            if dbg:
                S.dma("sp", gts[1, :, 0, :], win[:, 0, 0:128], reads=["win"], writes=["win_chk"])
            S.dma("sp", bs[:], bs_d, writes=["bs"])
            S.dma("sp", lng[:], vtm_d[2:3, :].to_broadcast([128, D]), writes=["lng"])
            sq = T["sq"]
            for t in range(NTILE):
                lat = t < SEQ // 128
                A1, B1 = mv["mix_l"] if lat else mv["mix_c"]
                S.dma("sp", x_t[:], xa[t * 128:(t + 1) * 128, :], writes=["x_t"])
                emit_rstd(S, x_t[:], "x_t", sq[:], T["ss"][:], T["rstd"][:], D)
                S.op("dve", lambda e: e.tensor_scalar(out=xn[:], in0=x_t[:], scalar1=T["rstd"][:, 0:1], scalar2=None, op0=ALU.mult),
                     reads=["x_t", "rstd"], writes=["xn"])
                S.group("pe", [(lambda e, k=k: e.transpose(out=pT[:, k, :], in_=xn[:, k * 128:(k + 1) * 128], identity=T["ident_b"][:]))
                               for k in range(KC)], reads=["xn", "ident_b"], writes=["pT"])
                S.op("dve", lambda e: e.tensor_tensor(out=sq[:].rearrange("p (k n) -> p k n", k=KC), in0=pT[:], in1=_b3(A1, 128), op=ALU.mult),
                     reads=["pT", "vec"], writes=["sq"])
                S.op("dve", lambda e: e.tensor_tensor(out=hT[:], in0=sq[:].rearrange("p (k n) -> p k n", k=KC), in1=_b3(B1, 128), op=ALU.add),
                     reads=["sq", "vec"], writes=["hT"])
                for j in range(8):
                    p = pU[j % 2]
                    S.group("pe", [(lambda e, k=k, p=p, j=j: e.matmul(p[:], hT[:, k, :], win[:, k, j * 512:(j + 1) * 512],
                                                                      start=(k == 0), stop=(k == KC - 1))) for k in range(KC)],
                            reads=["hT", "win", "win_chk"], writes=[f"pU{j % 2}"])
                    dst = u if j < 4 else v
                    dk = "u" if j < 4 else "v"
                    S.op("act", lambda e, p=p, dst=dst, j=j: e.activation(out=dst[:, (j % 4) * 512:(j % 4 + 1) * 512], in_=p[:], func=GELU_FUNC[0]),
                         reads=[f"pU{j % 2}"], writes=[dk])
                S.op("dve", lambda e: e.reduce_sum(out=mean[:], in_=v[:], axis=AX.X), reads=["v"], writes=["mean"])
                S.op("dve", lambda e: e.tensor_scalar(out=mean[:], in0=mean[:], scalar1=-1.0 / D, scalar2=None, op0=ALU.mult),
                     reads=["mean"], writes=["mean"])
                S.op("dve", lambda e: e.tensor_scalar(out=v[:], in0=v[:], scalar1=mean[:, 0:1], scalar2=None, op0=ALU.add),
                     reads=["v", "mean"], writes=["v"])
                if dbg and t == 0:
                    S.dma("sp", dbo["v"], v[:], reads=["v"], writes=["dbg"])
                emit_rstd(S, v[:], "v", sq[:], T["ss"][:], T["rstd"][:], D)
                S.op("dve", lambda e: e.tensor_scalar(out=vn[:], in0=v[:], scalar1=T["rstd"][:, 0:1], scalar2=None, op0=ALU.mult),
                     reads=["v", "rstd"], writes=["vn"])
                S.group("pe", [(lambda e, g=g: e.matmul(pS[:, g * 256:(g + 1) * 256], wsT[:, g, :], vn[:, g * 256:(g + 1) * 256],
                                                        start=True, stop=True)) for g in range(8)],
                        reads=["vn", "wsT"], writes=["pS"])
                S.op("dve", lambda e: e.tensor_tensor(out=sq[:], in0=pS[:], in1=lng[:], op=ALU.mult), reads=["pS", "lng"], writes=["sq"])
                S.op("dve", lambda e: e.tensor_tensor(out=sq[:].rearrange("p (g c) -> p g c", g=8), in0=sq[:].rearrange("p (g c) -> p g c", g=8),
                                                      in1=_b3(bs[:], 256), op=ALU.add), reads=["sq", "bs"], writes=["sq"])
                if dbg and t == 0:
                    S.dma("sp", dbo["mix"], sq[:], reads=["sq"], writes=["dbg"])
                S.op("dve", lambda e: e.tensor_tensor(out=gated[:], in0=sq[:], in1=u[:], op=ALU.mult), reads=["sq", "u"], writes=["gated"])
                S.group("pe", [(lambda e, k=k: e.transpose(out=pT[:, k, :], in_=gated[:, k * 128:(k + 1) * 128], identity=T["ident_b"][:]))
                               for k in range(KC)], reads=["gated", "ident_b"], writes=["pT"])
                if dbg and t == 0:
                    S.dma("sp", dbo["gated"], gated[:], reads=["gated"], writes=["dbg"])
                    S.dma("sp", dbo["vn"], vn[:], reads=["vn"], writes=["dbg"])
                    S.dma("sp", dbo["u"], u[:], reads=["u"], writes=["dbg"])
                    S.dma("sp", dbo["hT"], hT[:], reads=["hT"], writes=["dbg"])
                    S.dma("sp", dbo["xn"], xn[:], reads=["xn"], writes=["dbg"])
                    S.dma("sp", dbo["w0"], win[:, :, 0:128], reads=["win"], writes=["dbg"])
                    S.dma("sp", dbo["w2"], win[:, :, 2048:2176], reads=["win"], writes=["dbg"])
                S.op("act", lambda e: e.activation(out=gT[:], in_=pT[:], func=ACTF.Copy), reads=["pT"], writes=["gT"])
                S.dma("sp", gts[t], gT[:], reads=["gT"], writes=[f"gts{t}"])
        S.barrier()
        with contextlib.ExitStack() as st:
            sb = lambda n, sh, dt: st.enter_context(nc.sbuf_tensor(_uq("sb_" + n), sh, dt))
            ps = lambda n, sh, dt: st.enter_context(nc.psum_tensor(_uq("ps_" + n), sh, dt))
            wout = sb("wout", [128, KC, D], BF16)
            G = [sb(f"G{i}", [128, D], F32) for i in range(2)]
            x1 = sb("x1_s", [128, D], F32)
            alloc_ffn_prep(nc, st, S, T, wr_d, br_d, identf_d, vtm_d, {"l": (3, 4, 5), "c": (3, 6, 7)})
            pY = ps("pY", [128, D], F32)
            T["pT32"] = ps("pT32", [128, KC, 128], F32)
            T["pR"] = pY[:, 0:NEXP]
            for c2 in range(2):
                S.dma("pool", wout[:, :, c2 * 1024:(c2 + 1) * 1024], w_out[:, :, c2 * 1024:(c2 + 1) * 1024], writes=["wout"])
            for i in range(2):
                S.dma("sp", G[i][:], vtm_d[i:i + 1, :].to_broadcast([128, D]), writes=[f"G{i}"])
            for t in range(NTILE):
                lat = t < SEQ // 128
                A2, B2 = mv["ffn_l"] if lat else mv["ffn_c"]
                gi = 0 if lat else 1
                S.dma("sp", x_t[:], xa[t * 128:(t + 1) * 128, :], writes=["x_t"])
                S.dma("sp", gT[:], gts[t], reads=[f"gts{t}"], writes=["gT"])
                for j in range(4):
                    S.group("pe", [(lambda e, k=k, j=j: e.matmul(pY[:, j * 512:(j + 1) * 512], gT[:, k, :], wout[:, k, j * 512:(j + 1) * 512],
                                                                 start=(k == 0), stop=(k == KC - 1))) for k in range(KC)],
                            reads=["gT", "wout"], writes=["pR"])
                S.op("dve", lambda e, gi=gi: e.tensor_tensor(out=x1[:], in0=pY[:], in1=G[gi][:], op=ALU.mult), reads=["pR", f"G{gi}"], writes=["x1"])
                S.op("dve", lambda e: e.tensor_tensor(out=x1[:], in0=x1[:], in1=x_t[:], op=ALU.add), reads=["x1", "x_t"], writes=["x1"])
                S.dma("sp", x1_o[t * 128:(t + 1) * 128, :], x1[:], reads=["x1"], writes=["x1_o"])
                emit_ffn_prep(S, T, x1, "x1", A2, B2, t * 128, hT_o, gates_o, var="l" if lat else "c")
        S.finish()
    return nc


def _fm(vec):
    return np.ascontiguousarray(vec.reshape(KC, 128).T)


def _rows_fm(w):
    return np.ascontiguousarray(w.reshape(KC, 128, -1).transpose(1, 0, 2))


def gmlp_inputs(b, x, ctx, mod0, norm_mix_g, norm_ffn_g, gmlp_w_in, gmlp_norm_g, gmlp_w_s, gmlp_b_s, gmlp_w_out, w_router, b_router):
    ml, mc = mod0[b], mod0[8]
    vfm = np.stack([_fm(norm_mix_g), _fm(ml[0]), _fm(ml[1]), _fm(mc[0]), _fm(mc[1]),
                    _fm(norm_ffn_g), _fm(ml[3]), _fm(ml[4]), _fm(mc[3]), _fm(mc[4])], axis=1)
    return {
        "xa": np.ascontiguousarray(np.concatenate([x[b], ctx[b]], axis=0)),
        "w_in": _rows_fm(gmlp_w_in), "w_out": _rows_fm(gmlp_w_out),
        "wsT": np.ascontiguousarray(gmlp_w_s.transpose(2, 0, 1)),
        "vfm": np.ascontiguousarray(vfm.astype(np.float32)),
        "vtm": np.ascontiguousarray(np.stack([ml[2], mc[2], gmlp_norm_g, norm_ffn_g, ml[3], ml[4], mc[3], mc[4]]).astype(np.float32)),
        "bs": np.ascontiguousarray(gmlp_b_s.T),
        "wr": _rows_fm(w_router), "br": np.ascontiguousarray(b_router[None, :]),
        "identf": np.eye(128, dtype=np.float32), "identb": np.eye(128, dtype=np.float32).astype(ml_dtypes.bfloat16),
    }


MT = 1024


def build_moe(NT):
    nc = _new_nc()
    din = lambda n, sh, dt=F32: nc.dram_tensor(n, sh, dt, kind="ExternalInput").ap()
    hT_d = din("hT", [128, KC, NT], BF16)
    g_d = din("gts", [128, NT // 128, EPC])
    wg_d = din("wg", [EPC, KC, 128, KC, 128])
    wl_d = din("wl", [EPC, KC, 128, KC, 128])
    bgl_d = din("bgl", [128, EPC, 2, KC])
    wd_d = din("wd", [EPC, 4, 128, KC, 512])
    bd_d = din("bd", [EPC, D])
    y_o = nc.dram_tensor("ypart", [NT, D], F32, kind="ExternalOutput").ap()
    with contextlib.ExitStack() as st:
        S = Sched(nc, st)
        sb = lambda n, sh, dt: st.enter_context(nc.sbuf_tensor(_uq("sb_" + n), sh, dt))
        ps = lambda n, sh, dt: st.enter_context(nc.psum_tensor(_uq("ps_" + n), sh, dt))
        HT = sb("HT", [128, KC, MT], BF16)
        actT = sb("actT", [128, KC, MT], BF16)
        yacc = sb("yacc", [128, MT // 128, D], F32)
        wgl = [sb(f"wgl{i}", [128, 2, KC, 128], BF16) for i in range(2)]
        wdb = [sb(f"wdb{i}", [128, KC, 512], BF16) for i in range(2)]
        bd = sb("bd", [128, D], F32)
        gts = sb("gts", [128, NT // 128, EPC], F32)
        bgl = sb("bgl", [128, EPC, 2, KC], F32)
        glu = [sb(f"glu{i}", [128, 512], F32) for i in range(2)]
        sig = [sb(f"sig{i}", [128, 512], F32) for i in range(2)]
        lin = [sb(f"lin{i}", [128, 512], F32) for i in range(2)]
        tmp = [sb(f"tmp{i}", [128, 512], F32) for i in range(2)]
        pg = [ps(f"pg{i}", [128, 512], F32) for i in range(2)]
        pl = [ps(f"pl{i}", [128, 512], F32) for i in range(2)]
        po = [ps(f"po{i}", [128, 512], F32) for i in range(2)]
        S.dma("sp", gts[:], g_d, writes=["gts"])
        S.dma("sp", bgl[:], bgl_d, writes=["bgl"])
        ci = 0
        di = 0
        gi = 0
        oi = 0
        for s in range(NT // MT):
            S.dma("sp", HT[:], hT_d[:, :, s * MT:(s + 1) * MT], writes=["HT"])
            for e in range(EPC):
                S.dma("sp", bd[:], bd_d[e:e + 1, :].to_broadcast([128, D]), writes=["bd"])
                for j in range(KC):
                    w = wgl[ci % 2]
                    wk = f"wgl{ci % 2}"
                    ci += 1
                    S.dma("pool", w[:, 0, :, :], wg_d[e, j], writes=[wk])
                    S.dma("pool", w[:, 1, :, :], wl_d[e, j], writes=[wk])
                    for tg in range(MT // 512):
                        b = gi % 2
                        gi += 1
                        S.group("pe", [(lambda e_, k=k, w=w, b=b, tg=tg: e_.matmul(pg[b][:], w[:, 0, k, :], HT[:, k, tg * 512:(tg + 1) * 512],
                                                                                   start=(k == 0), stop=(k == KC - 1))) for k in range(KC)],
                                reads=[wk, "HT"], writes=[f"pg{b}"])
                        S.group("pe", [(lambda e_, k=k, w=w, b=b, tg=tg: e_.matmul(pl[b][:], w[:, 1, k, :], HT[:, k, tg * 512:(tg + 1) * 512],
                                                                                   start=(k == 0), stop=(k == KC - 1))) for k in range(KC)],
                                reads=[wk, "HT"], writes=[f"pl{b}"])
                        S.op("dve", lambda e_, b=b, e=e, j=j: e_.tensor_scalar(out=glu[b][:], in0=pg[b][:], scalar1=bgl[:, e, 0, j:j + 1], scalar2=LIMIT,
                                                                          op0=ALU.add, op1=ALU.min), reads=[f"pg{b}", "bgl"], writes=[f"glu{b}"])
                        S.op("act", lambda e_, b=b: e_.activation(out=sig[b][:], in_=glu[b][:], func=ACTF.Sigmoid, scale=ALPHA),
                             reads=[f"glu{b}"], writes=[f"sig{b}"])
                        S.op("dve", lambda e_, b=b, e=e, j=j: e_.tensor_scalar(out=lin[b][:], in0=pl[b][:], scalar1=bgl[:, e, 1, j:j + 1], scalar2=LIMIT,
                                                                          op0=ALU.add, op1=ALU.min), reads=[f"pl{b}", "bgl"], writes=[f"lin{b}"])
                        S.op("dve", lambda e_, b=b: e_.tensor_scalar(out=lin[b][:], in0=lin[b][:], scalar1=-LIMIT, scalar2=1.0,
                                                                     op0=ALU.max, op1=ALU.add), reads=[f"lin{b}"], writes=[f"lin{b}"])
                        S.op("dve", lambda e_, b=b: e_.tensor_tensor(out=glu[b][:], in0=glu[b][:], in1=sig[b][:], op=ALU.mult),
                             reads=[f"glu{b}", f"sig{b}"], writes=[f"glu{b}"])
                        S.op("dve", lambda e_, b=b, j=j, tg=tg: e_.tensor_tensor(out=actT[:, j, tg * 512:(tg + 1) * 512], in0=glu[b][:], in1=lin[b][:], op=ALU.mult),
                             reads=[f"glu{b}", f"lin{b}"], writes=["actT"])
                for dg in range(4):
                    wd = wdb[di % 2]
                    dk = f"wdb{di % 2}"
                    di += 1
                    S.dma("pool", wd[:], wd_d[e, dg], writes=[dk])
                    for t in range(MT // 128):
                        b = oi % 2
                        oi += 1
                        S.group("pe", [(lambda e_, k=k, wd=wd, b=b, t=t: e_.matmul(po[b][:], actT[:, k, t * 128:(t + 1) * 128], wd[:, k, :],
                                                                                   start=(k == 0), stop=(k == KC - 1))) for k in range(KC)],
                                reads=["actT", dk], writes=[f"po{b}"])
                        S.op("dve", lambda e_, b=b, dg=dg: e_.tensor_tensor(out=tmp[b][:], in0=po[b][:], in1=bd[:, dg * 512:(dg + 1) * 512], op=ALU.add),
                             reads=[f"po{b}", "bd"], writes=[f"tmp{b}"])
                        gcol = gts[:, s * (MT // 128) + t, e:e + 1]
                        ysl = yacc[:, t, dg * 512:(dg + 1) * 512]
                        if e == 0:
                            S.op("dve", lambda e_, b=b, gcol=gcol, ysl=ysl: e_.tensor_scalar(out=ysl, in0=tmp[b][:], scalar1=gcol, scalar2=None, op0=ALU.mult),
                                 reads=[f"tmp{b}", "gts"], writes=["yacc"])
                        else:
                            S.op("dve", lambda e_, b=b, gcol=gcol, ysl=ysl: e_.scalar_tensor_tensor(out=ysl, in0=tmp[b][:], scalar=gcol, in1=ysl,
                                                                                                op0=ALU.mult, op1=ALU.add),
                                 reads=[f"tmp{b}", "gts", "yacc"], writes=["yacc"])
            S.dma("sp", y_o[s * MT:(s + 1) * MT, :].rearrange("(t p) d -> p t d", p=128), yacc[:], reads=["yacc"], writes=["y_o"])
        S.finish()
    return nc


def moe_weights(core, w_gu, b_gu, w_down, b_down):
    es = slice(core * EPC, (core + 1) * EPC)
    wgu = w_gu[es].reshape(EPC, KC, 128, KC, 128, 2)
    wg = np.ascontiguousarray(wgu[..., 0].transpose(0, 3, 2, 1, 4))
    wl = np.ascontiguousarray(wgu[..., 1].transpose(0, 3, 2, 1, 4))
    bgu = b_gu[es].reshape(EPC, KC, 128, 2)
    bgl = np.ascontiguousarray(bgu.transpose(2, 0, 3, 1))
    wdf = np.ascontiguousarray(w_down[es].reshape(EPC, KC, 128, D).transpose(0, 2, 1, 3))
    return {"wg": wg, "wl": wl, "bgl": bgl, "wdf": wdf, "bd": np.ascontiguousarray(b_down[es])}


def moe_gates(core, gates_all):
    NT = gates_all.shape[0]
    g = gates_all[:, core * EPC:(core + 1) * EPC].reshape(NT // 128, 128, EPC)
    return np.ascontiguousarray(g.transpose(1, 0, 2))


def emit_combine(S, x_t, xk, yp_t, ysum, gate_tile, gk):
    S.op("dve", lambda e: e.reduce_sum(out=ysum[:], in_=yp_t[:].rearrange("p c d -> p d c"), axis=AX.X), reads=["yp_t"], writes=["ysum"])
    S.op("dve", lambda e: e.tensor_tensor(out=ysum[:], in0=ysum[:], in1=gate_tile[:], op=ALU.mult), reads=["ysum", gk], writes=["ysum"])
    S.op("dve", lambda e: e.tensor_tensor(out=x_t[:], in0=x_t[:], in1=ysum[:], op=ALU.add), reads=["ysum", xk], writes=[xk])


def build_final():
    nc = _new_nc()
    din = lambda n, sh, dt=F32: nc.dram_tensor(n, sh, dt, kind="ExternalInput").ap()
    x3 = din("x3", [SEQ, D])
    yp = din("yp", [NCORE, SEQ, D])
    g_d = din("g5", [1, D])
    out = nc.dram_tensor("out", [SEQ, D], F32, kind="ExternalOutput").ap()
    with contextlib.ExitStack() as st:
        S = Sched(nc, st)
        sb = lambda n, sh, dt: st.enter_context(nc.sbuf_tensor(_uq("sb_" + n), sh, dt))
        x_t = [sb(f"x_t{i}", [128, D], F32) for i in range(2)]
        yp_t = sb("yp_t", [128, NCORE, D], F32)
        ysum = sb("ysum", [128, D], F32)
        G = sb("G", [128, D], F32)
        S.dma("sp", G[:], g_d.to_broadcast([128, D]), writes=["G"])
        for t in range(SEQ // 128):
            xt = x_t[t % 2]
            xk = f"x_t{t % 2}"
            S.dma("sp", xt[:], x3[t * 128:(t + 1) * 128, :], writes=[xk])
            S.dma("sp", yp_t[:], yp[:, t * 128:(t + 1) * 128, :].rearrange("c p d -> p c d"), writes=["yp_t"])
            emit_combine(S, xt, xk, yp_t, ysum, G, "G")
            S.dma("sp", out[t * 128:(t + 1) * 128, :], xt[:], reads=[xk], writes=["out"])
        S.finish()
    return nc


T_G5L, T_G5C, T_G2L, T_QG, T_KG, T_SG = range(6)
SCALE = HD ** -0.5


def build_attn(lam_init):
    nc = _new_nc()
    din = lambda n, sh, dt=F32: nc.dram_tensor(n, sh, dt, kind="ExternalInput").ap()
    x1_d = din("x1", [NTOK, D])
    yp_d = din("yp", [NCORE, NTOK, D])
    vtm_d = din("vtm", [9, D])
    vfm_d = din("vfm", [128, NVF, KC])
    wqkv_d = din("wqkv", [128, KC, 3 * D])
    wo_d = din("wo", [128, KC, D])
    cs_d = din("cs", [SEQ, 2, 2, 32])
    lam_d = din("lam4", [1, 4, HD])
    wr_d = din("wr", [128, KC, NEXP])
    br_d = din("br", [1, NEXP])
    identf_d = din("identf", [128, 128])
    identb_d = din("identb", [128, 128], BF16)
    x3_o = nc.dram_tensor("x3", [SEQ, D], F32, kind="ExternalOutput").ap()
    hT_o = nc.dram_tensor("htm", [SEQ, D], BF16, kind="ExternalOutput").ap()
    gates_o = nc.dram_tensor("gates", [SEQ, NEXP], F32, kind="ExternalOutput").ap()
    x2s = nc.dram_tensor("x2s", [NTOK, D], F32).ap()
    hTs = nc.dram_tensor("hTs", [NTILE, 128, KC, 128], BF16).ap()
    qTs = nc.dram_tensor("qTs", [SEQ // 128, 128, NH2, 128], BF16).ap()
    kTs = nc.dram_tensor("kTs", [NTILE, 128, NH2, 128], BF16).ap()
    vs = nc.dram_tensor("vs", [NTILE, 128, D], BF16).ap()
    osd = nc.dram_tensor("osd", [SEQ // 128, 128, D], BF16).ap()
    NLT = SEQ // 128
    with contextlib.ExitStack() as st0:
        S = Sched(nc, st0)
        T = {}
        sb0 = lambda n, sh, dt: st0.enter_context(nc.sbuf_tensor(_uq("sb_" + n), sh, dt))
        vfm = sb0("vfm", [128, NVF, KC], F32)
        for nm in ("mix_l", "mix_c", "ffn_l", "ffn_c"):
            T["A_" + nm] = sb0("A_" + nm, [128, KC], F32)
        T["ident_b"] = sb0("ident_b", [128, 128], BF16)
        T["sq"] = sb0("sq", [128, D], F32)
        T["ss"] = sb0("ss", [128, 1], F32)
        T["rstd"] = sb0("rstd", [128, 1], F32)
        S.dma("sp", vfm[:], vfm_d, writes=["vec"])
        S.dma("sp", T["ident_b"][:], identb_d, writes=["ident_b"])
        mv = emit_modvecs(S, T, vfm)
        sq = T["sq"]
        with contextlib.ExitStack() as st:
            sb = lambda n, sh, dt: st.enter_context(nc.sbuf_tensor(_uq("sb_" + n), sh, dt))
            ps = lambda n, sh, dt: st.enter_context(nc.psum_tensor(_uq("ps_" + n), sh, dt))
            x_t = sb("x_t", [128, D], F32)
            hT = sb("hT_s", [128, KC, 128], BF16)
            yp_t = sb("yp_t", [128, NCORE, D], F32)
            ysum = sb("ysum", [128, D], F32)
            G5 = [sb(f"G5{i}", [128, D], F32) for i in range(2)]
            xn = sb("xn", [128, D], BF16)
            pT = ps("pT", [128, KC, 128], BF16)
            for i in range(2):
                S.dma("sp", G5[i][:], vtm_d[i:i + 1, :].to_broadcast([128, D]), writes=[f"G5{i}"])
            for t in range(NTILE):
                lat = t < NLT
                A1, B1 = mv["mix_l"] if lat else mv["mix_c"]
                gi = 0 if lat else 1
                S.dma("sp", x_t[:], x1_d[t * 128:(t + 1) * 128, :], writes=["x_t"])
                S.dma("sp", yp_t[:], yp_d[:, t * 128:(t + 1) * 128, :].rearrange("c p d -> p c d"), writes=["yp_t"])
                emit_combine(S, x_t, "x_t", yp_t, ysum, G5[gi], f"G5{gi}")
                S.dma("sp", x2s[t * 128:(t + 1) * 128, :], x_t[:], reads=["x_t"], writes=[f"x2s{t}"])
                emit_rstd(S, x_t[:], "x_t", sq[:], T["ss"][:], T["rstd"][:], D)
                S.op("dve", lambda e: e.tensor_scalar(out=xn[:], in0=x_t[:], scalar1=T["rstd"][:, 0:1], scalar2=None, op0=ALU.mult),
                     reads=["x_t", "rstd"], writes=["xn"])
                S.group("pe", [(lambda e, k=k: e.transpose(out=pT[:, k, :], in_=xn[:, k * 128:(k + 1) * 128], identity=T["ident_b"][:]))
                               for k in range(KC)], reads=["xn", "ident_b"], writes=["pT"])
                S.op("dve", lambda e: e.tensor_tensor(out=sq[:].rearrange("p (k n) -> p k n", k=KC), in0=pT[:], in1=_b3(A1, 128), op=ALU.mult),
                     reads=["pT", "vec"], writes=["sq"])
                S.op("dve", lambda e: e.tensor_tensor(out=hT[:], in0=sq[:].rearrange("p (k n) -> p k n", k=KC), in1=_b3(B1, 128), op=ALU.add),
                     reads=["sq", "vec"], writes=["hT"])
                S.dma("sp", hTs[t], hT[:], reads=["hT"], writes=[f"hTs{t}"])
        S.barrier()
        with contextlib.ExitStack() as st:
            sb = lambda n, sh, dt: st.enter_context(nc.sbuf_tensor(_uq("sb_" + n), sh, dt))
            ps = lambda n, sh, dt: st.enter_context(nc.psum_tensor(_uq("ps_" + n), sh, dt))
            wp = sb("wp", [128, KC, D], BF16)
            hT = sb("hT_s", [128, KC, 128], BF16)
            QG = sb("QG", [128, D], F32)
            KG = sb("KG", [128, D], F32)
            qn = sb("qn", [128, D], F32)
            qr = sb("qr", [128, D], BF16)
            qT = sb("qT", [128, NH2, 128], BF16)
            cs = sb("cs", [128, 2, 2, 32], F32)
            ta = sb("ta", [128, NH2 * 2 * 32], F32)
            tb = sb("tb", [128, NH2 * 2 * 32], F32)
            ss16 = sb("ss16", [128, NH2], F32)
            pQ = ps("pQ", [128, D], F32)
            pT = ps("pT", [128, NH2, 128], BF16)
            S.dma("sp", QG[:], vtm_d[T_QG:T_QG + 1, :].to_broadcast([128, D]), writes=["QG"])
            S.dma("sp", KG[:], vtm_d[T_KG:T_KG + 1, :].to_broadcast([128, D]), writes=["KG"])
            for part in range(3):
                for c2 in range(2):
                    S.dma("pool", wp[:, :, c2 * 1024:(c2 + 1) * 1024], wqkv_d[:, :, part * D + c2 * 1024:part * D + (c2 + 1) * 1024],
                          writes=["wp"])
                tiles = range(NLT) if part == 0 else range(NTILE)
                for t in tiles:
                    lat = t < NLT
                    S.dma("sp", hT[:], hTs[t], reads=[f"hTs{t}"], writes=["hT"])
                    for j in range(4):
                        S.group("pe", [(lambda e, k=k, j=j: e.matmul(pQ[:, j * 512:(j + 1) * 512], hT[:, k, :], wp[:, k, j * 512:(j + 1) * 512],
                                                                     start=(k == 0), stop=(k == KC - 1))) for k in range(KC)],
                                reads=["hT", "wp"], writes=["pQ"])
                    if part == 2:
                        S.op("act", lambda e: e.activation(out=qr[:], in_=pQ[:], func=ACTF.Copy), reads=["pQ"], writes=["qr"])
                        S.dma("sp", vs[t], qr[:], reads=["qr"], writes=[f"vs{t}"])
                        continue
                    GT, gk = (QG, "QG") if part == 0 else (KG, "KG")
                    S.op("act", lambda e: e.activation(out=sq[:], in_=pQ[:], func=ACTF.Square), reads=["pQ"], writes=["sq"])
                    S.op("dve", lambda e: e.reduce_sum(out=ss16[:], in_=sq[:].rearrange("p (h d) -> p h d", h=NH2), axis=AX.X),
                         reads=["sq"], writes=["ss16"])
                    S.op("act", lambda e: e.activation(out=ss16[:], in_=ss16[:], func=ACTF.Sqrt, scale=1.0 / HD, bias=EPS),
                         reads=["ss16"], writes=["ss16"])
                    S.op("dve", lambda e: e.reciprocal(out=ss16[:], in_=ss16[:]), reads=["ss16"], writes=["ss16"])
                    S.op("dve", lambda e: e.tensor_tensor(out=qn[:].rearrange("p (h d) -> p h d", h=NH2), in0=pQ[:].rearrange("p (h d) -> p h d", h=NH2),
                                                          in1=_b3(ss16[:], HD), op=ALU.mult), reads=["pQ", "ss16"], writes=["qn"])
                    if lat:
                        S.op("dve", lambda e, GT=GT: e.tensor_tensor(out=qn[:], in0=qn[:], in1=GT[:], op=ALU.mult), reads=["qn", gk], writes=["qn"])
                        S.dma("sp", cs[:], cs_d[t * 128:(t + 1) * 128], writes=["cs"])
                        v5 = lambda ap: ap.rearrange("p (h a f r) -> p h a f r", h=NH2, a=2, f=2, r=32)
                        x1v, x2v = v5(qn[:])[:, :, :, 0, :], v5(qn[:])[:, :, :, 1, :]
                        o1v, o2v = v5(qr[:])[:, :, :, 0, :], v5(qr[:])[:, :, :, 1, :]
                        cosb = cs[:, 0, :, :].unsqueeze(1).to_broadcast([128, NH2, 2, 32])
                        sinb = cs[:, 1, :, :].unsqueeze(1).to_broadcast([128, NH2, 2, 32])
                        tav = ta[:].rearrange("p (h a r) -> p h a r", h=NH2, a=2)
                        tbv = tb[:].rearrange("p (h a r) -> p h a r", h=NH2, a=2)
                        S.op("dve", lambda e: e.tensor_tensor(out=tav, in0=x1v, in1=cosb, op=ALU.mult), reads=["qn", "cs"], writes=["ta"])
                        S.op("pool", lambda e: e.tensor_tensor(out=tbv, in0=x2v, in1=sinb, op=ALU.mult), reads=["qn", "cs"], writes=["tb"])
                        S.op("dve", lambda e: e.tensor_tensor(out=o1v, in0=tav, in1=tbv, op=ALU.subtract), reads=["ta", "tb"], writes=["qr"])
                        S.op("dve", lambda e: e.tensor_tensor(out=tav, in0=x2v, in1=cosb, op=ALU.mult), reads=["qn", "cs"], writes=["ta"])
                        S.op("pool", lambda e: e.tensor_tensor(out=tbv, in0=x1v, in1=sinb, op=ALU.mult), reads=["qn", "cs"], writes=["tb"])
                        S.op("dve", lambda e: e.tensor_tensor(out=o2v, in0=tav, in1=tbv, op=ALU.add), reads=["ta", "tb"], writes=["qr"])
                    else:
                        S.op("dve", lambda e, GT=GT: e.tensor_tensor(out=qr[:], in0=qn[:], in1=GT[:], op=ALU.mult), reads=["qn", gk], writes=["qr"])
                    S.group("pe", [(lambda e, h=h: e.transpose(out=pT[:, h, :], in_=qr[:, h * 128:(h + 1) * 128], identity=T["ident_b"][:]))
                                   for h in range(NH2)], reads=["qr", "ident_b"], writes=["pT"])
                    S.op("act", lambda e: e.activation(out=qT[:], in_=pT[:], func=ACTF.Copy), reads=["pT"], writes=["qT"])
                    if part == 0:
                        S.dma("sp", qTs[t], qT[:], reads=["qT"], writes=[f"qTs{t}"])
                    else:
                        S.dma("sp", kTs[t], qT[:], reads=["qT"], writes=[f"kTs{t}"])
        S.barrier()
        with contextlib.ExitStack() as st:
            sb = lambda n, sh, dt: st.enter_context(nc.sbuf_tensor(_uq("sb_" + n), sh, dt))
            ps = lambda n, sh, dt: st.enter_context(nc.psum_tensor(_uq("ps_" + n), sh, dt))
            kT = sb("kT", [128, NH2, NTOK], BF16)
            va = sb("va", [128, NTILE, D], BF16)
            qTg = sb("qTg", [128, NH2, 512], BF16)
            PT = [sb(f"PT{i}", [128, 512], BF16) for i in range(2)]
            o_sb = sb("o_sb", [128, 4, D], BF16)
            o4 = sb("o4", [128, 4, 256], F32)
            o4b = sb("o4b", [128, 4, 256], F32)
            ss4 = sb("ss4", [128, 4], F32)
            rl4 = sb("rl4", [128, 4], F32)
            ones = sb("ones", [128, 1], BF16)
            onesf = sb("onesf", [1, 128], F32)
            SG = sb("SG", [128, D], F32)
            lam4 = sb("lam4", [1, 4, HD], F32)
            lt = sb("lt", [1, 2, HD], F32)
            l2 = sb("l2", [1, 2], F32)
            nlam = sb("nlam", [128, 1], F32)
            rs = sb("rs", [128, 8], F32)
            rl = sb("rl", [128, 1], F32)
            pS = [ps(f"pS{i}", [128, 512], F32) for i in range(2)]
            pO = ps("pO", [128, 8, 256], F32)
            pSm = ps("pSm", [128, 8], F32)
            for t in range(NTILE):
                S.dma("sp", kT[:, :, t * 128:(t + 1) * 128], kTs[t], reads=[f"kTs{t}"], writes=["kT"])
                S.dma("sp", va[:, t, :], vs[t], reads=[f"vs{t}"], writes=["va"])
            S.dma("sp", SG[:], vtm_d[T_SG:T_SG + 1, :].to_broadcast([128, D]), writes=["SG"])
            S.dma("sp", lam4[:], lam_d, writes=["lam4"])
            S.op("dve", lambda e: e.memset(ones[:], 1.0), writes=["ones"])
            S.op("dve", lambda e: e.memset(onesf[:], 1.0), writes=["onesf"])
            S.op("dve", lambda e: e.tensor_scalar(out=SG[:], in0=SG[:], scalar1=float(1.0 - lam_init), scalar2=None, op0=ALU.mult), reads=["SG"], writes=["SG"])
            S.op("dve", lambda e: e.tensor_tensor(out=lt[:, 0, :], in0=lam4[:, 0, :], in1=lam4[:, 1, :], op=ALU.mult), reads=["lam4"], writes=["lt"])
            S.op("dve", lambda e: e.tensor_tensor(out=lt[:, 1, :], in0=lam4[:, 2, :], in1=lam4[:, 3, :], op=ALU.mult), reads=["lam4", "lt"], writes=["lt"])
            S.op("dve", lambda e: e.reduce_sum(out=l2[:], in_=lt[:], axis=AX.X), reads=["lt"], writes=["l2"])
            S.op("act", lambda e: e.activation(out=l2[:], in_=l2[:], func=ACTF.Exp), reads=["l2"], writes=["l2"])
            S.op("dve", lambda e: e.tensor_tensor(out=l2[:, 0:1], in0=l2[:, 1:2], in1=l2[:, 0:1], op=ALU.subtract), reads=["l2"], writes=["l2"])
            S.op("dve", lambda e: e.tensor_scalar(out=l2[:, 0:1], in0=l2[:, 0:1], scalar1=-float(lam_init), scalar2=None, op0=ALU.add), reads=["l2"], writes=["l2"])
            S.op("pe", lambda e: e.matmul(pSm[:, 0:1], onesf[:], l2[:, 0:1], start=True, stop=True), reads=["onesf", "l2"], writes=["pSm"])
            S.op("dve", lambda e: e.tensor_copy(out=nlam[:], in_=pSm[:, 0:1]), reads=["pSm"], writes=["nlam"])
            si = 0
            for qg in range(4):
                for i in range(4):
                    S.dma("sp", qTg[:, :, i * 128:(i + 1) * 128], qTs[qg * 4 + i], reads=[f"qTs{qg * 4 + i}"], writes=["qTg"])
                for h in range(NH):
                    for j in range(2):
                        hh = 2 * h + j
                        for kc in range(NTILE):
                            b = si % 2
                            si += 1
                            S.op("pe", lambda e, b=b, hh=hh, kc=kc: e.matmul(pS[b][:], kT[:, hh, kc * 128:(kc + 1) * 128], qTg[:, hh, :], start=True, stop=True),
                                 reads=["kT", "qTg"], writes=[f"pS{b}"])
                            S.op("act", lambda e, b=b: e.activation(out=PT[b][:], in_=pS[b][:], func=ACTF.Exp, scale=SCALE),
                                 reads=[f"pS{b}"], writes=[f"PT{b}"])
                            fns = []
                            for qt in range(4):
                                fns.append(lambda e, b=b, qt=qt, j=j, kc=kc, h=h: e.matmul(pO[:, j * 4 + qt, :], PT[b][:, qt * 128:(qt + 1) * 128],
                                                                                         va[:, kc, h * 256:(h + 1) * 256], start=(kc == 0), stop=(kc == NTILE - 1)))
                                fns.append(lambda e, b=b, qt=qt, j=j, kc=kc: e.matmul(pSm[:, j * 4 + qt:j * 4 + qt + 1], PT[b][:, qt * 128:(qt + 1) * 128],
                                                                                    ones[:], start=(kc == 0), stop=(kc == NTILE - 1)))
                            S.group("pe", fns, reads=[f"PT{b}", "va", "ones"], writes=["pO", "pSm"])
                    S.op("dve", lambda e: e.reciprocal(out=rs[:], in_=pSm[:]), reads=["pSm"], writes=["rs"])
                    S.op("dve", lambda e: e.tensor_scalar(out=rl4[:], in0=rs[:, 4:8], scalar1=nlam[:, 0:1], scalar2=None, op0=ALU.mult),
                         reads=["rs", "nlam"], writes=["rl4"])
                    S.op("dve", lambda e: e.tensor_tensor(out=o4[:], in0=pO[:, 0:4, :], in1=_b3(rs[:, 0:4], 256), op=ALU.mult),
                         reads=["pO", "rs"], writes=["o4"])
                    S.op("dve", lambda e: e.tensor_tensor(out=o4b[:], in0=pO[:, 4:8, :], in1=_b3(rl4[:], 256), op=ALU.mult),
                         reads=["pO", "rl4"], writes=["o4b"])
                    S.op("dve", lambda e: e.tensor_tensor(out=o4[:], in0=o4[:], in1=o4b[:], op=ALU.add), reads=["o4", "o4b"], writes=["o4"])
                    S.op("act", lambda e: e.activation(out=o4b[:], in_=o4[:], func=ACTF.Square), reads=["o4"], writes=["o4b"])
                    S.op("dve", lambda e: e.reduce_sum(out=ss4[:], in_=o4b[:], axis=AX.X), reads=["o4b"], writes=["ss4"])
                    S.op("act", lambda e: e.activation(out=ss4[:], in_=ss4[:], func=ACTF.Sqrt, scale=1.0 / 256, bias=EPS), reads=["ss4"], writes=["ss4"])
                    S.op("dve", lambda e: e.reciprocal(out=ss4[:], in_=ss4[:]), reads=["ss4"], writes=["ss4"])
                    S.op("dve", lambda e: e.tensor_tensor(out=o4[:], in0=o4[:], in1=_b3(ss4[:], 256), op=ALU.mult), reads=["o4", "ss4"], writes=["o4"])
                    S.op("dve", lambda e, h=h: e.tensor_tensor(out=o_sb[:, :, h * 256:(h + 1) * 256], in0=o4[:],
                                                               in1=SG[:, h * 256:(h + 1) * 256].unsqueeze(1).to_broadcast([128, 4, 256]), op=ALU.mult),
                         reads=["o4", "SG"], writes=["o_sb"])
                for i in range(4):
                    S.dma("sp", osd[qg * 4 + i], o_sb[:, i, :], reads=["o_sb"], writes=[f"osd{qg * 4 + i}"])
        S.barrier()
        with contextlib.ExitStack() as st:
            sb = lambda n, sh, dt: st.enter_context(nc.sbuf_tensor(_uq("sb_" + n), sh, dt))
            ps = lambda n, sh, dt: st.enter_context(nc.psum_tensor(_uq("ps_" + n), sh, dt))
            wo = sb("wo", [128, KC, D], BF16)
            x_t = sb("x_t", [128, D], F32)
            G2 = sb("G2", [128, D], F32)
            o_t = sb("o_t", [128, D], BF16)
            oT = sb("oT", [128, KC, 128], BF16)
            x3 = sb("x3_s", [128, D], F32)
            alloc_ffn_prep(nc, st, S, T, wr_d, br_d, identf_d, vtm_d, {"l": (6, 7, 8)})
            pY = ps("pY", [128, D], F32)
            T["pT32"] = ps("pT32", [128, KC, 128], F32)
            T["pR"] = pY[:, 0:NEXP]
            pTb = T["pT32"][:].bitcast(BF16)[:, 0:KC, 0:128] if False else None
            for c2 in range(2):
                S.dma("pool", wo[:, :, c2 * 1024:(c2 + 1) * 1024], wo_d[:, :, c2 * 1024:(c2 + 1) * 1024], writes=["wo"])
            S.dma("sp", G2[:], vtm_d[T_G2L:T_G2L + 1, :].to_broadcast([128, D]), writes=["G2"])
            A2, B2 = mv["ffn_l"]
            for t in range(NLT):
                S.dma("sp", o_t[:], osd[t], reads=[f"osd{t}"], writes=["o_t"])
                S.dma("sp", x_t[:], x2s[t * 128:(t + 1) * 128, :], reads=[f"x2s{t}"], writes=["x_t"])
                pYb = pY[:].bitcast(BF16).rearrange("p (k n) -> p k n", n=128)
                S.group("pe", [(lambda e, k=k: e.transpose(out=pYb[:, k, :], in_=o_t[:, k * 128:(k + 1) * 128], identity=T["ident_b"][:]))
                               for k in range(KC)], reads=["o_t", "ident_b"], writes=["pR"])
                S.op("act", lambda e: e.activation(out=oT[:], in_=pYb[:, 0:KC, :], func=ACTF.Copy), reads=["pR"], writes=["oT"])
                for j in range(4):
                    S.group("pe", [(lambda e, k=k, j=j: e.matmul(pY[:, j * 512:(j + 1) * 512], oT[:, k, :], wo[:, k, j * 512:(j + 1) * 512],
                                                                 start=(k == 0), stop=(k == KC - 1))) for k in range(KC)],
                            reads=["oT", "wo"], writes=["pR"])
                S.op("dve", lambda e: e.tensor_tensor(out=x3[:], in0=pY[:], in1=G2[:], op=ALU.mult), reads=["pR", "G2"], writes=["x3"])
                S.op("dve", lambda e: e.tensor_tensor(out=x3[:], in0=x3[:], in1=x_t[:], op=ALU.add), reads=["x3", "x_t"], writes=["x3"])
                S.dma("sp", x3_o[t * 128:(t + 1) * 128, :], x3[:], reads=["x3"], writes=["x3_o"])
                emit_ffn_prep(S, T, x3, "x3", A2, B2, t * 128, hT_o, gates_o)
        S.finish()
    return nc


def rope_tables():
    rows = SEQ // 64
    row_pos = np.repeat(np.arange(rows), 64).astype(np.float32)
    col_pos = np.tile(np.arange(64), rows).astype(np.float32)
    inv = (10000.0 ** (-np.arange(32, dtype=np.float32) / 32)).astype(np.float32)
    ang = np.stack([row_pos[:, None] * inv, col_pos[:, None] * inv], axis=1)
    return np.ascontiguousarray(np.stack([np.cos(ang), np.sin(ang)], axis=1).astype(np.float32))


def attn_inputs(b, x1, yp, mod0, mod1, norm_mix_g, norm_ffn_g, w_qkv, q_g, k_g, lam4, subln_g, w_o, w_router, b_router, cs):
    ml0, mc0 = mod0[b], mod0[8]
    ml, mc = mod1[b], mod1[8]
    vfm = np.stack([_fm(norm_mix_g), _fm(ml[0]), _fm(ml[1]), _fm(mc[0]), _fm(mc[1]),
                    _fm(norm_ffn_g), _fm(ml[3]), _fm(ml[4]), _fm(mc[3]), _fm(mc[4])], axis=1)
    vtm = np.stack([ml0[5], mc0[5], ml[2], np.tile(q_g, NH2), np.tile(k_g, NH2), np.tile(subln_g, NH),
                    norm_ffn_g, ml[3], ml[4]]).astype(np.float32)
    return {
        "x1": x1, "yp": yp, "vtm": np.ascontiguousarray(vtm), "vfm": np.ascontiguousarray(vfm.astype(np.float32)),
        "wqkv": _rows_fm(w_qkv), "wo": _rows_fm(w_o), "cs": cs, "lam4": np.ascontiguousarray(lam4[None]),
        "wr": _rows_fm(w_router), "br": np.ascontiguousarray(b_router[None, :]),
        "identf": np.eye(128, dtype=np.float32), "identb": np.eye(128, dtype=np.float32).astype(ml_dtypes.bfloat16),
    }


def _run(nc, in_maps):
    res = run_bass_kernel_spmd(nc, in_maps, core_ids=list(range(NCORE)))
    return res.results


def _run_moe(NT, hT_list, gates_list, w_gu, b_gu, w_down, b_down):
    hT_all = np.ascontiguousarray(np.concatenate(hT_list, axis=2))
    gates_all = np.concatenate(gates_list, axis=0)
    ims = []
    for c in range(NCORE):
        m = moe_weights(c, w_gu, b_gu, w_down, b_down)
        m["hT"] = hT_all
        m["gts"] = moe_gates(c, gates_all)
        ims.append(m)
    res = _run(build_moe(NT), ims)
    return [np.asarray(r["ypart"]) for r in res]


def kernel(x, c, ctx, c_ctx, w_mod, b_mod, norm_mix_g, norm_ffn_g,
           gmlp_w_in, gmlp_norm_g, gmlp_w_s, gmlp_b_s, gmlp_w_out,
           diff_w_qkv, diff_q_norm_g, diff_k_norm_g, diff_lambda, diff_subln_g, diff_w_o,
           moe_w_router, moe_b_router, moe_w_gate_up, moe_b_gate_up, moe_w_down, moe_b_down):
    f = lambda a: np.asarray(a, dtype=np.float32)
    x, c, ctx, c_ctx, w_mod, b_mod = f(x), f(c), f(ctx), f(c_ctx), f(w_mod), f(b_mod)
    norm_mix_g, norm_ffn_g = f(norm_mix_g), f(norm_ffn_g)
    moe_w_gate_up, moe_b_gate_up, moe_w_down, moe_b_down = f(moe_w_gate_up), f(moe_b_gate_up), f(moe_w_down), f(moe_b_down)
    moe_w_router, moe_b_router = f(moe_w_router), f(moe_b_router)
    mod = run_mod(c, c_ctx, w_mod, b_mod)
    ims = [gmlp_inputs(b, x, ctx, mod[0], norm_mix_g[0], norm_ffn_g[0], f(gmlp_w_in)[0], f(gmlp_norm_g)[0], f(gmlp_w_s)[0],
                       f(gmlp_b_s)[0], f(gmlp_w_out)[0], moe_w_router[0], moe_b_router[0]) for b in range(NCORE)]
    resA = _run(build_gmlp(), ims)
    x1 = [np.asarray(r["x1"]) for r in resA]
    yp0 = _run_moe2(NCORE * NTOK, [np.asarray(r["htm"]) for r in resA], [np.asarray(r["gates"]) for r in resA],
                   moe_w_gate_up[0], moe_b_gate_up[0], moe_w_down[0], moe_b_down[0])
    del resA
    import math
    lam_init = 0.8 - 0.6 * math.exp(-0.3 * 1)
    cs = rope_tables()
    ims = []
    for b in range(NCORE):
        yp = np.ascontiguousarray(np.stack([yp0[cc][b * NTOK:(b + 1) * NTOK] for cc in range(NCORE)], axis=0))
        ims.append(attn_inputs(b, x1[b], yp, mod[0], mod[1], norm_mix_g[1], norm_ffn_g[1], f(diff_w_qkv)[0], f(diff_q_norm_g)[0],
                               f(diff_k_norm_g)[0], f(diff_lambda)[0], f(diff_subln_g)[0], f(diff_w_o)[0],
                               moe_w_router[1], moe_b_router[1], cs))
    del yp0
    resC = _run(build_attn(lam_init), ims)
    del ims
    x3 = [np.asarray(r["x3"]) for r in resC]
    yp1 = _run_moe2(NCORE * SEQ, [np.asarray(r["htm"]) for r in resC], [np.asarray(r["gates"]) for r in resC],
                   moe_w_gate_up[1], moe_b_gate_up[1], moe_w_down[1], moe_b_down[1])
    del resC
    ims = []
    for b in range(NCORE):
        yp = np.ascontiguousarray(np.stack([yp1[cc][b * SEQ:(b + 1) * SEQ] for cc in range(NCORE)], axis=0))
        ims.append({"x3": x3[b], "yp": yp, "g5": np.ascontiguousarray(mod[1][b][5][None, :])})
    del yp1
    resE = _run(build_final(), ims)
    return np.stack([np.asarray(r["out"]) for r in resE], axis=0).astype(np.float32)


I32 = mybir.dt.int32
CAP = 4096
MT2 = 2048
DW = 256
BIGIDX = 1.0e6


def _ind_dma(S, nc, reads, writes, **kw):
    S._deps("pool", reads, writes)
    lst = S.dma_sems["pool"]
    ent = lst[S.dma_rr["pool"] % len(lst)]
    S.dma_rr["pool"] += 1
    if ent[2] > 0:
        S._wait("pool", (ent[0], ent[1], ent[2], "dma"))
    ins = nc.gpsimd.indirect_dma_start(**kw)
    ent[2] += 16
    ins.then_inc(ent[0], 16)
    S._record((ent[0], ent[1], ent[2], "dma"), reads, writes)


def build_moe2(NT):
    F = NT // 128
    NB = CAP // 128
    NROW = NT + 128
    nc = _new_nc()
    din = lambda n, sh, dt=F32: nc.dram_tensor(n, sh, dt, kind="ExternalInput").ap()
    h_d = din("htm", [NROW, D], BF16)
    gpm_d = din("gpm", [128, EPC, F])
    gtm_d = din("gtm", [NROW, EPC])
    U_d = din("U", [128, 128])
    tid_d = din("tid", [128, F])
    pid_d = din("pid", [128, 1])
    identb_d = din("identb", [128, 128], BF16)
    wg_d = din("wg", [EPC, KC, 128, KC, 128])
    wl_d = din("wl", [EPC, KC, 128, KC, 128])
    bgl_d = din("bgl", [128, EPC, 2, KC])
    wd_d = din("wd8", [EPC, D // DW, 128, KC, DW])
    bd_d = din("bd", [EPC, D])
    y_o = [nc.dram_tensor(f"ypart{dg}", [NROW, DW], F32, kind="ExternalOutput").ap() for dg in range(D // DW)]
    lists = [nc.dram_tensor(f"lists{ex}", [CAP, 2], I32).ap() for ex in range(EPC)]
    with contextlib.ExitStack() as st:
        S = Sched(nc, st)
        sb = lambda n, sh, dt: st.enter_context(nc.sbuf_tensor(_uq("sb_" + n), sh, dt))
        ps = lambda n, sh, dt: st.enter_context(nc.psum_tensor(_uq("ps_" + n), sh, dt))
        XT = sb("XT", [128, KC, MT2], BF16)
        actT = sb("actT", [128, KC, MT2], BF16)
        wgl = [sb(f"wgl{i}", [128, 2, KC, 128], BF16) for i in range(2)]
        wdb = [sb(f"wdb{i}", [128, KC, DW], BF16) for i in range(2)]
        bd = sb("bd", [128, D], F32)
        bgl = sb("bgl", [128, EPC, 2, KC], F32)
        glu = [sb(f"glu{i}", [128, 512], F32) for i in range(2)]
        sig = [sb(f"sig{i}", [128, 512], F32) for i in range(2)]
        lin = [sb(f"lin{i}", [128, 512], F32) for i in range(2)]
        tmp = [sb(f"tmp{i}", [128, DW], F32) for i in range(4)]
        xg = [sb(f"xg{i}", [128, D], BF16) for i in range(2)]
        gsl = sb("gsl", [128, MT2 // 128, EPC], F32)
        identb = sb("identb", [128, 128], BF16)
        gpm = sb("gpm", [128, EPC, F], F32)
        U = sb("U", [128, 128], F32)
        tid = sb("tid", [128, F], F32)
        tid2 = sb("tid2", [128, F, 2], I32)
        pid = sb("pid", [128, 1], F32)
        m = sb("m", [128, F], F32)
        ca = sb("ca", [128, F], F32)
        cb = sb("cb", [128, F], F32)
        off = sb("off", [128, 1], F32)
        dest = sb("dest", [128, F], F32)
        desti = sb("desti", [128, F], I32)
        pref = sb("pref", [128, 1], F32)
        pre = sb("pre", [128, NB, 2], I32)
        lst = [sb(f"lst{e}", [128, NB, 2], I32) for e in range(EPC)]
        pT = ps("pT", [128, KC, 128], BF16)
        pg = [ps(f"pg{i}", [128, 512], F32) for i in range(2)]
        pl = [ps(f"pl{i}", [128, 512], F32) for i in range(2)]
        po = [ps(f"po{i}", [128, 512], F32) for i in range(2)]
        for n, t_, d_ in (("gpm", gpm, gpm_d), ("U", U, U_d), ("tid", tid, tid_d), ("pid", pid, pid_d), ("identb", identb, identb_d), ("bgl", bgl, bgl_d)):
            S.dma("sp", t_[:], d_, writes=[n])
        reg_cap = nc.gpsimd.alloc_register("bc_cap")
        nc.gpsimd.reg_mov(reg_cap, CAP - 1)
        reg_row = nc.gpsimd.alloc_register("bc_row")
        nc.gpsimd.reg_mov(reg_row, NROW - 1)
        S.op("dve", lambda e: e.memset(bd[:], 0.0), writes=["bd"])
        for dg in range(D // DW):
            for r in range(0, NROW, 1024):
                n = min(1024, NROW - r)
                S.dma("sp", y_o[dg][r:r + n, :].rearrange("(a p) d -> p a d", p=128), bd[:, 0:(n // 128) * DW].rearrange("p (a d) -> p a d", d=DW),
                      reads=["bd"], writes=["y0"])
        S.op("dve", lambda e: e.tensor_scalar(out=pref[:], in0=pid[:], scalar1=float(NT), scalar2=None, op0=ALU.add), reads=["pid"], writes=["pref"])
        S.op("dve", lambda e: e.tensor_copy(out=pre[:].rearrange("p b o -> p (b o)"), in_=pref[:, 0:1].to_broadcast([128, 2 * NB])), reads=["pref"], writes=["pre"])
        S.op("dve", lambda e: e.tensor_copy(out=tid2[:], in_=tid[:].unsqueeze(2).to_broadcast([128, F, 2])), reads=["tid"], writes=["tid2"])
        for ex in range(EPC):
            S.dma("sp", lists[ex].rearrange("(b p) o -> p b o", p=128), pre[:], reads=["pre"], writes=[f"list0_{ex}"])
            S.op("dve", lambda e, ex=ex: e.tensor_scalar(out=m[:], in0=gpm[:, ex, :], scalar1=0.0, scalar2=None, op0=ALU.is_gt), reads=["gpm"], writes=["m"])
            S.op("dve", lambda e: e.tensor_copy(out=ca[:], in_=m[:]), reads=["m"], writes=["ca"])
            cur, nxt, ck, nk = ca, cb, "ca", "cb"
            s_ = 1
            while s_ < F:
                S.op("dve", lambda e, cur=cur, nxt=nxt, s_=s_: e.tensor_copy(out=nxt[:, 0:s_], in_=cur[:, 0:s_]), reads=[ck], writes=[nk])
                S.op("dve", lambda e, cur=cur, nxt=nxt, s_=s_: e.tensor_tensor(out=nxt[:, s_:F], in0=cur[:, s_:F], in1=cur[:, 0:F - s_], op=ALU.add),
                     reads=[ck], writes=[nk])
                cur, nxt, ck, nk = nxt, cur, nk, ck
                s_ *= 2
            S.op("pe", lambda e, cur=cur: e.matmul(po[0][:, 0:1], U[:], cur[:, F - 1:F], start=True, stop=True), reads=[ck, "U"], writes=["po0"])
            S.op("dve", lambda e: e.tensor_scalar(out=off[:], in0=po[0][:, 0:1], scalar1=-1.0, scalar2=None, op0=ALU.add), reads=["po0"], writes=["off"])
            S.op("dve", lambda e, cur=cur: e.tensor_scalar(out=dest[:], in0=cur[:], scalar1=off[:, 0:1], scalar2=-BIGIDX, op0=ALU.add, op1=ALU.add),
                 reads=[ck, "off"], writes=["dest"])
            S.op("dve", lambda e: e.tensor_tensor(out=dest[:], in0=dest[:], in1=m[:], op=ALU.mult), reads=["dest", "m"], writes=["dest"])
            S.op("dve", lambda e: e.tensor_scalar(out=dest[:], in0=dest[:], scalar1=BIGIDX, scalar2=None, op0=ALU.add), reads=["dest"], writes=["dest"])
            S.op("dve", lambda e: e.tensor_copy(out=desti[:], in_=dest[:]), reads=["dest"], writes=["desti"])
            for f in range(F):
                _ind_dma(S, nc, ["desti", "tid2", f"list0_{ex}"], [f"lists_{ex}_{f}"], out=lists[ex],
                         out_offset=bass.IndirectOffsetOnAxis(ap=desti[:, f:f + 1], axis=0), in_=tid2[:, f, :], in_offset=None,
                         bounds_check=reg_cap, oob_is_err=False)
            S.dma("sp", lst[ex][:], lists[ex].rearrange("(b p) o -> p b o", p=128), reads=[f"lists_{ex}_{f}" for f in range(F)], writes=[f"lst{ex}"])
        ci = di = gi = oi = xi = 0
        for ex in range(EPC):
            S.dma("sp", bd[:], bd_d[ex:ex + 1, :].to_broadcast([128, D]), writes=["bd"])
            for s in range(CAP // MT2):
                for blk in range(MT2 // 128):
                    gb = s * (MT2 // 128) + blk
                    x_ = xg[xi % 2]
                    xk = f"xg{xi % 2}"
                    xi += 1
                    _ind_dma(S, nc, [f"lst{ex}"], [xk], out=x_[:], out_offset=None, in_=h_d,
                             in_offset=bass.IndirectOffsetOnAxis(ap=lst[ex][:, gb, 0:1], axis=0))
                    _ind_dma(S, nc, [f"lst{ex}"], ["gsl"], out=gsl[:, blk, :], out_offset=None, in_=gtm_d,
                             in_offset=bass.IndirectOffsetOnAxis(ap=lst[ex][:, gb, 0:1], axis=0))
                    S.group("pe", [(lambda e, k=k, x_=x_: e.transpose(out=pT[:, k, :], in_=x_[:, k * 128:(k + 1) * 128], identity=identb[:]))
                                   for k in range(KC)], reads=[xk, "identb"], writes=["pT"])
                    S.op("act", lambda e, blk=blk: e.activation(out=XT[:, :, blk * 128:(blk + 1) * 128], in_=pT[:], func=ACTF.Copy),
                         reads=["pT"], writes=["XT"])
                for j in range(KC):
                    w = wgl[ci % 2]
                    wk = f"wgl{ci % 2}"
                    ci += 1
                    S.dma("pool", w[:, 0, :, :], wg_d[ex, j], writes=[wk])
                    S.dma("pool", w[:, 1, :, :], wl_d[ex, j], writes=[wk])
                    for tg in range(MT2 // 512):
                        b = gi % 2
                        gi += 1
                        S.group("pe", [(lambda e_, k=k, w=w, b=b, tg=tg: e_.matmul(pg[b][:], w[:, 0, k, :], XT[:, k, tg * 512:(tg + 1) * 512],
                                                                                   start=(k == 0), stop=(k == KC - 1))) for k in range(KC)],
                                reads=[wk, "XT"], writes=[f"pg{b}"])
                        S.group("pe", [(lambda e_, k=k, w=w, b=b, tg=tg: e_.matmul(pl[b][:], w[:, 1, k, :], XT[:, k, tg * 512:(tg + 1) * 512],
                                                                                   start=(k == 0), stop=(k == KC - 1))) for k in range(KC)],
                                reads=[wk, "XT"], writes=[f"pl{b}"])
                        S.op("dve", lambda e_, b=b, ex=ex, j=j: e_.tensor_scalar(out=glu[b][:], in0=pg[b][:], scalar1=bgl[:, ex, 0, j:j + 1], scalar2=LIMIT,
                                                                            op0=ALU.add, op1=ALU.min), reads=[f"pg{b}", "bgl"], writes=[f"glu{b}"])
                        S.op("act", lambda e_, b=b: e_.activation(out=sig[b][:], in_=glu[b][:], func=ACTF.Sigmoid, scale=ALPHA),
                             reads=[f"glu{b}"], writes=[f"sig{b}"])
                        S.op("dve", lambda e_, b=b, ex=ex, j=j: e_.tensor_scalar(out=lin[b][:], in0=pl[b][:], scalar1=bgl[:, ex, 1, j:j + 1], scalar2=LIMIT,
                                                                            op0=ALU.add, op1=ALU.min), reads=[f"pl{b}", "bgl"], writes=[f"lin{b}"])
                        S.op("dve", lambda e_, b=b: e_.tensor_scalar(out=lin[b][:], in0=lin[b][:], scalar1=-LIMIT, scalar2=1.0,
                                                                     op0=ALU.max, op1=ALU.add), reads=[f"lin{b}"], writes=[f"lin{b}"])
                        S.op("dve", lambda e_, b=b: e_.tensor_tensor(out=glu[b][:], in0=glu[b][:], in1=sig[b][:], op=ALU.mult),
                             reads=[f"glu{b}", f"sig{b}"], writes=[f"glu{b}"])
                        S.op("dve", lambda e_, b=b, j=j, tg=tg: e_.tensor_tensor(out=actT[:, j, tg * 512:(tg + 1) * 512], in0=glu[b][:], in1=lin[b][:], op=ALU.mult),
                             reads=[f"glu{b}", f"lin{b}"], writes=["actT"])
                wd_next = None
                for dg in range(D // DW):
                    if wd_next is None:
                        wd, dk = wdb[di % 2], f"wdb{di % 2}"
                        di += 1
                        S.dma("pool", wd[:], wd_d[ex, dg], writes=[dk])
                    else:
                        wd, dk = wd_next
                    if dg < D // DW - 1:
                        wd_next = (wdb[di % 2], f"wdb{di % 2}")
                        di += 1
                        S.dma("pool", wd_next[0][:], wd_d[ex, dg + 1], writes=[wd_next[1]])
                    for t in range(MT2 // 128):
                        b = oi % 2
                        tb = oi % 4
                        oi += 1
                        gb = s * (MT2 // 128) + t
                        S.group("pe", [(lambda e_, k=k, wd=wd, b=b, t=t: e_.matmul(po[b][:, 0:DW], actT[:, k, t * 128:(t + 1) * 128], wd[:, k, :],
                                                                                   start=(k == 0), stop=(k == KC - 1))) for k in range(KC)],
                                reads=["actT", dk], writes=[f"po{b}"])
                        S.op("dve", lambda e_, b=b, tb=tb, dg=dg: e_.tensor_tensor(out=tmp[tb][:], in0=po[b][:, 0:DW], in1=bd[:, dg * DW:(dg + 1) * DW], op=ALU.add),
                             reads=[f"po{b}", "bd"], writes=[f"tmp{tb}"])
                        S.op("dve", lambda e_, tb=tb, t=t, ex=ex: e_.tensor_scalar(out=tmp[tb][:], in0=tmp[tb][:], scalar1=gsl[:, t, ex:ex + 1], scalar2=None, op0=ALU.mult),
                             reads=[f"tmp{tb}", "gsl"], writes=[f"tmp{tb}"])
                        _ind_dma(S, nc, [f"tmp{tb}", f"lst{ex}", "y0", f"y_{ex - 1}"], [f"y_{ex}"], out=y_o[dg],
                                 out_offset=bass.IndirectOffsetOnAxis(ap=lst[ex][:, gb, 0:1], axis=0), in_=tmp[tb][:], in_offset=None,
                                 compute_op=ALU.add, oob_is_err=True, bounds_check=reg_row)
        S.finish()
    return nc


MT3 = 1024


def build_moe3(NT):
    F = NT // 128
    NB = CAP // 128
    NROW = NT + 128
    nc = _new_nc()
    din = lambda n, sh, dt=F32: nc.dram_tensor(n, sh, dt, kind="ExternalInput").ap()
    h_d = din("htm", [NROW, D], BF16)
    gpm_d = din("gpm", [128, EPC, F])
    U_d = din("U", [128, 128])
    tid_d = din("tid", [128, F])
    pid_d = din("pid", [128, 1])
    identb_d = din("identb", [128, 128], BF16)
    wg_d = din("wg", [EPC, KC, 128, KC, 128])
    wl_d = din("wl", [EPC, KC, 128, KC, 128])
    bgl_d = din("bgl", [128, EPC, 2, KC])
    wd_d = din("wdf", [EPC, 128, KC, D])
    bd_d = din("bd", [EPC, D])
    y_o = nc.dram_tensor("ypart", [NROW, D], F32, kind="ExternalOutput").ap()
    lists = [nc.dram_tensor(f"lists{ex}", [CAP, 2], I32).ap() for ex in range(EPC)]
    with contextlib.ExitStack() as st:
        S = Sched(nc, st)
        sb = lambda n, sh, dt: st.enter_context(nc.sbuf_tensor(_uq("sb_" + n), sh, dt))
        ps = lambda n, sh, dt: st.enter_context(nc.psum_tensor(_uq("ps_" + n), sh, dt))
        XT = sb("XT", [128, KC, MT3], BF16)
        actT = sb("actT", [128, KC, MT3], BF16)
        wgl = [sb(f"wgl{i}", [128, 2, KC, 128], BF16) for i in range(2)]
        wdf = sb("wdf", [128, KC, D], BF16)
        yblk = [sb(f"yblk{i}", [128, D], F32) for i in range(2)]
        bd = sb("bd", [128, D], F32)
        bgl = sb("bgl", [128, EPC, 2, KC], F32)
        glu = [sb(f"glu{i}", [128, 512], F32) for i in range(2)]
        sig = [sb(f"sig{i}", [128, 512], F32) for i in range(2)]
        lin = [sb(f"lin{i}", [128, 512], F32) for i in range(2)]
        tmp = [sb(f"tmp{i}", [128, 512], F32) for i in range(2)]
        xg = [sb(f"xg{i}", [128, D], BF16) for i in range(2)]
        identb = sb("identb", [128, 128], BF16)
        gpm = sb("gpm", [128, EPC, F], F32)
        U = sb("U", [128, 128], F32)
        tid = sb("tid", [128, F], F32)
        tid2 = sb("tid2", [128, F, 2], I32)
        pid = sb("pid", [128, 1], F32)
        m = sb("m", [128, F], F32)
        ca = sb("ca", [128, F], F32)
        cb = sb("cb", [128, F], F32)
        off = sb("off", [128, 1], F32)
        dest = sb("dest", [128, F], F32)
        desti = sb("desti", [128, F], I32)
        pref = sb("pref", [128, 1], F32)
        pre = sb("pre", [128, NB, 2], I32)
        lst = [sb(f"lst{e}", [128, NB, 2], I32) for e in range(EPC)]
        pT = ps("pT", [128, KC, 128], BF16)
        pg = [ps(f"pg{i}", [128, 512], F32) for i in range(2)]
        pl = [ps(f"pl{i}", [128, 512], F32) for i in range(2)]
        po = [ps(f"po{i}", [128, 512], F32) for i in range(2)]
        for n, t_, d_ in (("gpm", gpm, gpm_d), ("U", U, U_d), ("tid", tid, tid_d), ("pid", pid, pid_d), ("identb", identb, identb_d), ("bgl", bgl, bgl_d)):
            S.dma("sp", t_[:], d_, writes=[n])
        reg_cap = nc.gpsimd.alloc_register("bc_cap")
        nc.gpsimd.reg_mov(reg_cap, CAP - 1)
        reg_row = nc.gpsimd.alloc_register("bc_row")
        nc.gpsimd.reg_mov(reg_row, NROW - 1)
        S.op("dve", lambda e: e.memset(bd[:], 0.0), writes=["bd"])
        for r in range(0, NROW, 1024):
            n = min(1024, NROW - r)
            S.dma("sp", y_o[r:r + n, :].rearrange("(a p) d -> p a d", p=128), bd[:].unsqueeze(1).to_broadcast([128, n // 128, D]),
                  reads=["bd"], writes=["y0"])
        S.op("dve", lambda e: e.tensor_scalar(out=pref[:], in0=pid[:], scalar1=float(NT), scalar2=None, op0=ALU.add), reads=["pid"], writes=["pref"])
        S.op("dve", lambda e: e.memset(pre[:], 0), writes=["pre"])
        S.op("dve", lambda e: e.tensor_copy(out=pre[:, :, 0], in_=pref[:, 0:1].to_broadcast([128, NB])), reads=["pref", "pre"], writes=["pre"])
        S.op("dve", lambda e: e.tensor_copy(out=tid2[:, :, 0], in_=tid[:]), reads=["tid"], writes=["tid2"])
        for ex in range(EPC):
            S.dma("sp", lists[ex].rearrange("(b p) o -> p b o", p=128), pre[:], reads=["pre"], writes=[f"list0_{ex}"])
            S.op("dve", lambda e, ex=ex: e.tensor_copy(out=tid2[:, :, 1], in_=gpm[:, ex, :].bitcast(I32)), reads=["gpm", "tid2"], writes=["tid2"])
            S.op("dve", lambda e, ex=ex: e.tensor_scalar(out=m[:], in0=gpm[:, ex, :], scalar1=0.0, scalar2=None, op0=ALU.is_gt), reads=["gpm"], writes=["m"])
            S.op("dve", lambda e: e.tensor_copy(out=ca[:], in_=m[:]), reads=["m"], writes=["ca"])
            cur, nxt, ck, nk = ca, cb, "ca", "cb"
            s_ = 1
            while s_ < F:
                S.op("dve", lambda e, cur=cur, nxt=nxt, s_=s_: e.tensor_copy(out=nxt[:, 0:s_], in_=cur[:, 0:s_]), reads=[ck], writes=[nk])
                S.op("dve", lambda e, cur=cur, nxt=nxt, s_=s_: e.tensor_tensor(out=nxt[:, s_:F], in0=cur[:, s_:F], in1=cur[:, 0:F - s_], op=ALU.add),
                     reads=[ck], writes=[nk])
                cur, nxt, ck, nk = nxt, cur, nk, ck
                s_ *= 2
            S.op("pe", lambda e, cur=cur: e.matmul(po[0][:, 0:1], U[:], cur[:, F - 1:F], start=True, stop=True), reads=[ck, "U"], writes=["po0"])
            S.op("dve", lambda e: e.tensor_scalar(out=off[:], in0=po[0][:, 0:1], scalar1=-1.0, scalar2=None, op0=ALU.add), reads=["po0"], writes=["off"])
            S.op("dve", lambda e, cur=cur: e.tensor_scalar(out=dest[:], in0=cur[:], scalar1=off[:, 0:1], scalar2=-BIGIDX, op0=ALU.add, op1=ALU.add),
                 reads=[ck, "off"], writes=["dest"])
            S.op("dve", lambda e: e.tensor_tensor(out=dest[:], in0=dest[:], in1=m[:], op=ALU.mult), reads=["dest", "m"], writes=["dest"])
            S.op("dve", lambda e: e.tensor_scalar(out=dest[:], in0=dest[:], scalar1=BIGIDX, scalar2=None, op0=ALU.add), reads=["dest"], writes=["dest"])
            S.op("dve", lambda e: e.tensor_copy(out=desti[:], in_=dest[:]), reads=["dest"], writes=["desti"])
            for f in range(F):
                _ind_dma(S, nc, ["desti", "tid2", f"list0_{ex}"], [f"lists_{ex}_{f}"], out=lists[ex],
                         out_offset=bass.IndirectOffsetOnAxis(ap=desti[:, f:f + 1], axis=0), in_=tid2[:, f, :], in_offset=None,
                         bounds_check=reg_cap, oob_is_err=False)
            S.dma("sp", lst[ex][:], lists[ex].rearrange("(b p) o -> p b o", p=128), reads=[f"lists_{ex}_{f}" for f in range(F)], writes=[f"lst{ex}"])
        ci = di = gi = oi = xi = yi = 0
        for ex in range(EPC):
            S.dma("sp", bd[:], bd_d[ex:ex + 1, :].to_broadcast([128, D]), writes=["bd"])
            for c4 in range(4):
                S.dma("pool", wdf[:, :, c4 * 512:(c4 + 1) * 512], wd_d[ex, :, :, c4 * 512:(c4 + 1) * 512], writes=["wdf"])
            for s in range(CAP // MT3):
                for blk in range(MT3 // 128):
                    gb = s * (MT3 // 128) + blk
                    x_ = xg[xi % 2]
                    xk = f"xg{xi % 2}"
                    xi += 1
                    _ind_dma(S, nc, [f"lst{ex}"], [xk], out=x_[:], out_offset=None, in_=h_d,
                             in_offset=bass.IndirectOffsetOnAxis(ap=lst[ex][:, gb, 0:1], axis=0))
                    S.group("pe", [(lambda e, k=k, x_=x_: e.transpose(out=pT[:, k, :], in_=x_[:, k * 128:(k + 1) * 128], identity=identb[:]))
                                   for k in range(KC)], reads=[xk, "identb"], writes=["pT"])
                    S.op("act", lambda e, blk=blk: e.activation(out=XT[:, :, blk * 128:(blk + 1) * 128], in_=pT[:], func=ACTF.Copy),
                         reads=["pT"], writes=["XT"])
                for j in range(KC):
                    w = wgl[ci % 2]
                    wk = f"wgl{ci % 2}"
                    ci += 1
                    S.dma("pool", w[:, 0, :, :], wg_d[ex, j], writes=[wk])
                    S.dma("pool", w[:, 1, :, :], wl_d[ex, j], writes=[wk])
                    for tg in range(MT3 // 512):
                        b = gi % 2
                        gi += 1
                        S.group("pe", [(lambda e_, k=k, w=w, b=b, tg=tg: e_.matmul(pg[b][:], w[:, 0, k, :], XT[:, k, tg * 512:(tg + 1) * 512],
                                                                                   start=(k == 0), stop=(k == KC - 1))) for k in range(KC)],
                                reads=[wk, "XT"], writes=[f"pg{b}"])
                        S.group("pe", [(lambda e_, k=k, w=w, b=b, tg=tg: e_.matmul(pl[b][:], w[:, 1, k, :], XT[:, k, tg * 512:(tg + 1) * 512],
                                                                                   start=(k == 0), stop=(k == KC - 1))) for k in range(KC)],
                                reads=[wk, "XT"], writes=[f"pl{b}"])
                        S.op("dve", lambda e_, b=b, ex=ex, j=j: e_.tensor_scalar(out=glu[b][:], in0=pg[b][:], scalar1=bgl[:, ex, 0, j:j + 1], scalar2=LIMIT,
                                                                            op0=ALU.add, op1=ALU.min), reads=[f"pg{b}", "bgl"], writes=[f"glu{b}"])
                        S.op("act", lambda e_, b=b: e_.activation(out=sig[b][:], in_=glu[b][:], func=ACTF.Sigmoid, scale=ALPHA),
                             reads=[f"glu{b}"], writes=[f"sig{b}"])
                        S.op("dve", lambda e_, b=b, ex=ex, j=j: e_.tensor_scalar(out=lin[b][:], in0=pl[b][:], scalar1=bgl[:, ex, 1, j:j + 1], scalar2=LIMIT,
                                                                            op0=ALU.add, op1=ALU.min), reads=[f"pl{b}", "bgl"], writes=[f"lin{b}"])
                        S.op("dve", lambda e_, b=b: e_.tensor_scalar(out=lin[b][:], in0=lin[b][:], scalar1=-LIMIT, scalar2=1.0,
                                                                     op0=ALU.max, op1=ALU.add), reads=[f"lin{b}"], writes=[f"lin{b}"])
                        S.op("dve", lambda e_, b=b: e_.tensor_tensor(out=glu[b][:], in0=glu[b][:], in1=sig[b][:], op=ALU.mult),
                             reads=[f"glu{b}", f"sig{b}"], writes=[f"glu{b}"])
                        S.op("dve", lambda e_, b=b, j=j, tg=tg: e_.tensor_tensor(out=actT[:, j, tg * 512:(tg + 1) * 512], in0=glu[b][:], in1=lin[b][:], op=ALU.mult),
                             reads=[f"glu{b}", f"lin{b}"], writes=["actT"])
                for t in range(MT3 // 128):
                    gb = s * (MT3 // 128) + t
                    yb = yblk[yi % 2]
                    yk = f"yblk{yi % 2}"
                    yi += 1
                    gcol = lst[ex][:, gb, 1:2].bitcast(F32)
                    for dg in range(4):
                        b = oi % 2
                        oi += 1
                        S.group("pe", [(lambda e_, k=k, b=b, t=t, dg=dg: e_.matmul(po[b][:], actT[:, k, t * 128:(t + 1) * 128], wdf[:, k, dg * 512:(dg + 1) * 512],
                                                                                   start=(k == 0), stop=(k == KC - 1))) for k in range(KC)],
                                reads=["actT", "wdf"], writes=[f"po{b}"])
                        S.op("dve", lambda e_, b=b, dg=dg: e_.tensor_tensor(out=tmp[b][:], in0=po[b][:], in1=bd[:, dg * 512:(dg + 1) * 512], op=ALU.add),
                             reads=[f"po{b}", "bd"], writes=[f"tmp{b}"])
                        S.op("dve", lambda e_, b=b, dg=dg, yb=yb, gcol=gcol: e_.tensor_scalar(out=yb[:, dg * 512:(dg + 1) * 512], in0=tmp[b][:], scalar1=gcol, scalar2=None, op0=ALU.mult),
                             reads=[f"tmp{b}", f"lst{ex}"], writes=[yk])
                    _ind_dma(S, nc, [yk, f"lst{ex}", "y0", f"y_{ex - 1}"], [f"y_{ex}"], out=y_o,
                             out_offset=bass.IndirectOffsetOnAxis(ap=lst[ex][:, gb, 0:1], axis=0), in_=yb[:], in_offset=None,
                             compute_op=ALU.add, oob_is_err=True, bounds_check=reg_row)
        S.finish()
    return nc


def moe2_inputs(core, htm_all, gates_all, NT):
    F = NT // 128
    g = gates_all[:, core * EPC:(core + 1) * EPC]
    gpm = np.ascontiguousarray(g.reshape(128, F, EPC).transpose(0, 2, 1))
    tid = (np.arange(128)[:, None] * F + np.arange(F)[None, :]).astype(np.float32)
    return {"htm": htm_all, "gpm": gpm, "U": np.triu(np.ones((128, 128), np.float32), 1),
            "tid": tid, "pid": np.arange(128, dtype=np.float32)[:, None],
            "identb": np.eye(128, dtype=np.float32).astype(ml_dtypes.bfloat16)}


def _run_moe2(NT, htm_list, gates_list, w_gu, b_gu, w_down, b_down):
    htm_all = np.ascontiguousarray(np.concatenate(htm_list + [np.zeros((128, D), ml_dtypes.bfloat16)], axis=0))
    gates_all = np.concatenate(gates_list, axis=0)
    ims = []
    for c in range(NCORE):
        m = moe_weights(c, w_gu, b_gu, w_down, b_down)
        m.update(moe2_inputs(c, htm_all, gates_all, NT))
        ims.append(m)
    res = _run(build_moe3(NT), ims)
    return [np.asarray(r["ypart"])[:NT] for r in res]
```

```python
import contextlib
import numpy as np
import ml_dtypes
import concourse.bass as bass
import concourse.mybir as mybir
from concourse.bass_utils import run_bass_kernel_spmd

F32 = mybir.dt.float32
BF16 = mybir.dt.bfloat16
ALU = mybir.AluOpType
ACTF = mybir.ActivationFunctionType
AX = mybir.AxisListType

D = 2048
KC = 16
NCORE = 8
SEQ = 2048
CTX = 256
NTOK = SEQ + CTX
NTILE = NTOK // 128
NEXP = 32
EPC = 4
EPS = 1e-6
HD = 128
NH2 = 16
NH = 8
ALPHA = 1.702
LIMIT = 7.0
GELU_FUNC = [ACTF.Gelu]


class Sched:
    def __init__(self, nc, stack, n_dma=10):
        self.nc = nc
        self.eng = {"pe": nc.tensor, "act": nc.scalar, "dve": nc.vector, "pool": nc.gpsimd, "sp": nc.sync}
        self.sem = {}
        self.cnt = {}
        self.known = {k: {} for k in self.eng}
        for k in self.eng:
            self.sem[k] = stack.enter_context(nc.semaphore("s_" + k))
            self.cnt[k] = 0
        self.dma_sems = {}
        self.dma_rr = {}
        for q in ("sp", "pool", "act"):
            self.dma_sems[q] = [[stack.enter_context(nc.semaphore(f"d_{q}{i}")), f"d_{q}{i}", 0] for i in range(n_dma)]
            self.dma_rr[q] = 0
        self.last_w = {}
        self.readers = {}

    def _wait(self, e, tok):
        sem, key, val, owner = tok
        if self.known[e].get(key, 0) >= val:
            return
        self.eng[e].wait_ge(sem, val)
        self.known[e][key] = val

    def _deps(self, e, reads, writes):
        for b in reads:
            for t in self.last_w.get(b, {}).values():
                self._wait(e, t)
        for b in writes:
            for t in self.last_w.get(b, {}).values():
                if t[3] != e:
                    self._wait(e, t)
            for t in self.readers.get(b, {}).values():
                if t[3] != e:
                    self._wait(e, t)

    def _record(self, tok, reads, writes):
        for b in reads:
            self.readers.setdefault(b, {})[tok[1]] = tok
        for b in writes:
            prev = self.last_w.get(b, {})
            if tok[3] == "dma" and prev and all(t[3] == "dma" for t in prev.values()) and not self.readers.get(b):
                prev[tok[1]] = tok
            else:
                self.last_w[b] = {tok[1]: tok}
            self.readers[b] = {}

    def op(self, e, fn, reads=(), writes=()):
        self._deps(e, reads, writes)
        ins = fn(self.eng[e])
        self.cnt[e] += 1
        ins.then_inc(self.sem[e], 1)
        tok = (self.sem[e], e, self.cnt[e], e)
        self._record(tok, reads, writes)
        return tok

    def group(self, e, fns, reads=(), writes=()):
        self._deps(e, reads, writes)
        ins = None
        for fn in fns:
            ins = fn(self.eng[e])
        self.cnt[e] += 1
        ins.then_inc(self.sem[e], 1)
        tok = (self.sem[e], e, self.cnt[e], e)
        self._record(tok, reads, writes)
        return tok

    def dma(self, q, out, in_, reads=(), writes=(), **kw):
        self._deps(q, reads, writes)
        lst = self.dma_sems[q]
        ent = lst[self.dma_rr[q] % len(lst)]
        self.dma_rr[q] += 1
        if ent[2] > 0:
            self._wait(q, (ent[0], ent[1], ent[2], "dma"))
        ins = self.eng[q].dma_start(out=out, in_=in_, **kw)
        ent[2] += 16
        ins.then_inc(ent[0], 16)
        tok = (ent[0], ent[1], ent[2], "dma")
        self._record(tok, reads, writes)
        return tok

    def barrier(self):
        toks = [(self.sem[k], k, self.cnt[k], k) for k in self.eng if self.cnt[k] > 0]
        for q in self.dma_sems:
            for ent in self.dma_sems[q]:
                if ent[2] > 0:
                    toks.append((ent[0], ent[1], ent[2], "dma"))
        for e in self.eng:
            for t in toks:
                if t[3] != e:
                    self._wait(e, t)

    def finish(self):
        for q in self.dma_sems:
            for ent in self.dma_sems[q]:
                if ent[2] > 0:
                    self._wait("sp", (ent[0], ent[1], ent[2], "dma"))


_UQ = [0]


def _uq(n):
    _UQ[0] += 1
    return f"{n}_{_UQ[0]}"


def _new_nc():
    _UQ[0] = 0
    return bass.Bass("TRN2", target_bir_lowering=False)


def _bcast(ap, shape):
    return ap.to_broadcast(list(shape))


MCOLS = 6 * D // NCORE


def build_mod():
    nc = _new_nc()
    cT = nc.dram_tensor("cT", [128, KC, 9], F32, kind="ExternalInput").ap()
    wm = nc.dram_tensor("wm", [2, 128, KC, MCOLS], F32, kind="ExternalInput").ap()
    bm = nc.dram_tensor("bm", [2, MCOLS], F32, kind="ExternalInput").ap()
    out = nc.dram_tensor("mod", [2, 9, MCOLS], F32, kind="ExternalOutput").ap()
    with contextlib.ExitStack() as st:
        S = Sched(nc, st)
        sb = lambda n, sh, dt: st.enter_context(nc.sbuf_tensor(_uq("sb_" + n), sh, dt))
        cs = sb("cs", [128, KC, 9], F32)
        css = sb("css", [128, KC, 9], F32)
        wt = [sb(f"wt{i}", [128, KC, 512], F32) for i in range(2)]
        bt = sb("bt", [9, 2, MCOLS], F32)
        ot = [sb(f"ot{i}", [9, 512], F32) for i in range(2)]
        ps = [st.enter_context(nc.psum_tensor(f"ps{i}", [9, 512], F32)) for i in range(2)]
        S.dma("sp", cs[:], cT, writes=["cs"])
        for l in range(2):
            S.dma("sp", bt[:, l, :], bm[l:l + 1, :].partition_broadcast(9) if False else bm[l:l + 1, :].to_broadcast([9, MCOLS]),
                  writes=["bt"])
        S.op("act", lambda e: e.activation(out=css[:], in_=cs[:], func=ACTF.Silu), reads=["cs"], writes=["css"])
        i = 0
        for l in range(2):
            for g in range(MCOLS // 512):
                w = wt[i % 2]
                S.dma("sp", w[:], wm[l, :, :, g * 512:(g + 1) * 512], writes=[f"wt{i % 2}"])
                p = ps[i % 2]
                S.group("pe", [
                    (lambda e, k=k, w=w, p=p: e.matmul(p[:], css[:, k, :], w[:, k, :], start=(k == 0), stop=(k == KC - 1)))
                    for k in range(KC)], reads=["css", f"wt{i % 2}"], writes=[f"ps{i % 2}"])
                o = ot[i % 2]
                S.op("dve", lambda e, o=o, p=p, l=l, g=g: e.tensor_tensor(out=o[:], in0=p[:], in1=bt[:, l, g * 512:(g + 1) * 512], op=ALU.add),
                     reads=[f"ps{i % 2}", "bt"], writes=[f"ot{i % 2}"])
                S.dma("sp", out[l, :, g * 512:(g + 1) * 512], o[:], reads=[f"ot{i % 2}"], writes=["out"])
                i += 1
        S.finish()
    return nc


def run_mod(c, c_ctx, w_mod, b_mod):
    c_all = np.concatenate([c, c_ctx[None, :]], axis=0)
    cT = np.ascontiguousarray(c_all.T.reshape(KC, 128, 9).transpose(1, 0, 2))
    in_maps = []
    for core in range(NCORE):
        sl = slice(core * MCOLS, (core + 1) * MCOLS)
        wm = np.ascontiguousarray(w_mod[:, :, sl].reshape(2, KC, 128, MCOLS).transpose(0, 2, 1, 3))
        in_maps.append({"cT": cT, "wm": wm, "bm": np.ascontiguousarray(b_mod[:, sl])})
    nc = build_mod()
    res = run_bass_kernel_spmd(nc, in_maps, core_ids=list(range(NCORE)))
    mod = np.concatenate([r["mod"] for r in res.results], axis=2)
    return mod.reshape(2, 9, 6, D)


def _b3(ap2, n):
    return ap2.unsqueeze(2).to_broadcast([ap2.shape[0], ap2.shape[1], n])


def emit_rstd(S, x_t, xk, sq, ss, rstd, n):
    S.op("act", lambda e: e.activation(out=sq, in_=x_t, func=ACTF.Square, accum_out=ss), reads=[xk], writes=["sq", "ss"])
    S.op("act", lambda e: e.activation(out=rstd, in_=ss, func=ACTF.Sqrt, scale=1.0 / n, bias=EPS), reads=["ss"], writes=["rstd"])
    S.op("dve", lambda e: e.reciprocal(out=rstd, in_=rstd), reads=["rstd"], writes=["rstd"])


def emit_ffn_prep(S, T, x1, x1k, A2, B2, tok0, htm_out, gates_out, var="l"):
    emit_rstd(S, x1[:], x1k, T["sq"][:], T["ss"][:], T["rstd"][:], D)
    S.op("dve", lambda e: e.tensor_scalar(out=T["xnf"][:], in0=x1[:], scalar1=T["rstd"][:, 0:1], scalar2=None, op0=ALU.mult),
         reads=[x1k, "rstd"], writes=["xnf"])
    pT = T["pT32"]
    S.group("pe", [(lambda e, k=k: e.transpose(out=pT[:, k, :], in_=T["xnf"][:, k * 128:(k + 1) * 128], identity=T["ident_f"][:]))
                   for k in range(KC)], reads=["xnf", "ident_f"], writes=["pT32"])
    h32 = T["h32"]
    S.op("dve", lambda e: e.tensor_tensor(out=h32[:], in0=pT[:], in1=_b3(A2, 128), op=ALU.mult), reads=["pT32", "vec"], writes=["h32"])
    S.op("dve", lambda e: e.tensor_tensor(out=h32[:], in0=h32[:], in1=_b3(B2, 128), op=ALU.add), reads=["h32", "vec"], writes=["h32"])
    S.op("dve", lambda e: e.tensor_tensor(out=T["sq"][:], in0=T["xnf"][:], in1=T["A2tm_" + var][:], op=ALU.mult), reads=["xnf", "A2tm"], writes=["sq"])
    S.op("dve", lambda e: e.tensor_tensor(out=T["htmb"][:], in0=T["sq"][:], in1=T["B2tm_" + var][:], op=ALU.add), reads=["sq", "A2tm"], writes=["htmb"])
    S.dma("sp", htm_out[tok0:tok0 + 128, :], T["htmb"][:], reads=["htmb"], writes=["htm_out"])
    pr = T["pR"]
    S.group("pe", [(lambda e, k=k: e.matmul(pr[:], h32[:, k, :], T["wr"][:, k, :], start=(k == 0), stop=(k == KC - 1)))
                   for k in range(KC)], reads=["h32", "wr"], writes=["pR"])
    lg, m8, ex, msk = T["lg"], T["m8"], T["ex"], T["msk"]
    S.op("dve", lambda e: e.tensor_tensor(out=lg[:], in0=pr[:], in1=T["br"][:], op=ALU.add), reads=["pR", "br"], writes=["lg"])
    S.op("dve", lambda e: e.max(out=m8[:], in_=lg[:]), reads=["lg"], writes=["m8"])
    S.op("dve", lambda e: e.tensor_scalar(out=msk[:], in0=lg[:], scalar1=m8[:, 3:4], scalar2=None, op0=ALU.is_ge), reads=["lg", "m8"], writes=["msk"])
    S.op("dve", lambda e: e.tensor_scalar(out=T["nmx"][:], in0=m8[:, 0:1], scalar1=-1.0, scalar2=None, op0=ALU.mult), reads=["m8"], writes=["nmx"])
    S.op("act", lambda e: e.activation(out=ex[:], in_=lg[:], func=ACTF.Exp, bias=T["nmx"][:, 0:1]), reads=["lg", "nmx"], writes=["ex"])
    S.op("dve", lambda e: e.tensor_tensor(out=ex[:], in0=ex[:], in1=msk[:], op=ALU.mult), reads=["ex", "msk"], writes=["ex"])
    S.op("dve", lambda e: e.reduce_sum(out=T["sm"][:], in_=ex[:], axis=AX.X), reads=["ex"], writes=["sm"])
    S.op("dve", lambda e: e.reciprocal(out=T["sm"][:], in_=T["sm"][:]), reads=["sm"], writes=["sm"])
    S.op("dve", lambda e: e.tensor_scalar(out=T["gt"][:], in0=ex[:], scalar1=T["sm"][:, 0:1], scalar2=None, op0=ALU.mult), reads=["ex", "sm"], writes=["gt"])
    S.dma("sp", gates_out[tok0:tok0 + 128, :], T["gt"][:], reads=["gt"], writes=["gates_out"])


def alloc_ffn_prep(nc, st, S, T, wr_d, br_d, identf_d, vtm_d, variants):
    sb = lambda n, sh, dt: st.enter_context(nc.sbuf_tensor(_uq("sb_" + n), sh, dt))
    for n, sh, dt in (("sq", [128, D], F32), ("ss", [128, 1], F32), ("rstd", [128, 1], F32), ("xnf", [128, D], F32),
                      ("h32", [128, KC, 128], F32), ("htmb", [128, D], BF16), ("wr", [128, KC, NEXP], F32),
                      ("br", [128, NEXP], F32), ("ident_f", [128, 128], F32), ("lg", [128, NEXP], F32), ("m8", [128, 8], F32),
                      ("ex", [128, NEXP], F32), ("msk", [128, NEXP], F32), ("nmx", [128, 1], F32), ("sm", [128, 1], F32),
                      ("gt", [128, NEXP], F32)):
        if n not in T:
            T[n] = sb(n, sh, dt)
    S.dma("sp", T["wr"][:], wr_d, writes=["wr"])
    S.dma("sp", T["br"][:], br_d.to_broadcast([128, NEXP]), writes=["br"])
    S.dma("sp", T["ident_f"][:], identf_d, writes=["ident_f"])
    for v, (gr, shr, scr) in variants.items():
        A = T["A2tm_" + v] = sb("A2tm_" + v, [128, D], F32)
        B = T["B2tm_" + v] = sb("B2tm_" + v, [128, D], F32)
        S.dma("sp", A[:], vtm_d[gr:gr + 1, :].to_broadcast([128, D]), writes=["A2tm"])
        S.dma("sp", T["xnf"][:], vtm_d[scr:scr + 1, :].to_broadcast([128, D]), writes=["xnf"])
        S.dma("sp", B[:], vtm_d[shr:shr + 1, :].to_broadcast([128, D]), writes=["A2tm"])
        S.op("dve", lambda e, A=A: e.scalar_tensor_tensor(out=A[:], in0=T["xnf"][:], scalar=1.0, in1=A[:], op0=ALU.add, op1=ALU.mult),
             reads=["xnf", "A2tm"], writes=["A2tm"])


V_GMIX, V_SH_L, V_SC_L, V_SH_C, V_SC_C, V_GFFN, V_SH2_L, V_SC2_L, V_SH2_C, V_SC2_C = range(10)
NVF = 10


def emit_modvecs(S, T, vfm):
    res = {}
    for nm, g, sh, sc in (("mix_l", V_GMIX, V_SH_L, V_SC_L), ("mix_c", V_GMIX, V_SH_C, V_SC_C),
                          ("ffn_l", V_GFFN, V_SH2_L, V_SC2_L), ("ffn_c", V_GFFN, V_SH2_C, V_SC2_C)):
        a = T["A_" + nm]
        S.op("dve", lambda e, a=a, g=g, sc=sc: e.scalar_tensor_tensor(out=a[:], in0=vfm[:, sc, :], scalar=1.0, in1=vfm[:, g, :],
                                                                    op0=ALU.add, op1=ALU.mult), reads=["vec"], writes=["vec"])
        res[nm] = (a[:], vfm[:, sh, :])
    return res


def build_gmlp(dbg=False):
    nc = _new_nc()
    din = lambda n, sh, dt=F32: nc.dram_tensor(n, sh, dt, kind="ExternalInput").ap()
    xa = din("xa", [NTOK, D])
    w_in = din("w_in", [128, KC, 2 * D])
    w_out = din("w_out", [128, KC, D])
    wsT_d = din("wsT", [128, 8, 128])
    vfm_d = din("vfm", [128, NVF, KC])
    vtm_d = din("vtm", [8, D])
    bs_d = din("bs", [128, 8])
    wr_d = din("wr", [128, KC, NEXP])
    br_d = din("br", [1, NEXP])
    identf_d = din("identf", [128, 128])
    identb_d = din("identb", [128, 128], BF16)
    x1_o = nc.dram_tensor("x1", [NTOK, D], F32, kind="ExternalOutput").ap()
    hT_o = nc.dram_tensor("htm", [NTOK, D], BF16, kind="ExternalOutput").ap()
    gates_o = nc.dram_tensor("gates", [NTOK, NEXP], F32, kind="ExternalOutput").ap()
    gts = nc.dram_tensor("gts", [NTILE, 128, KC, 128], BF16).ap()
    if dbg:
        dbo = {n: nc.dram_tensor("dbg_" + n, sh, dt, kind="ExternalOutput").ap() for n, sh, dt in (
            ("hT", [128, KC, 128], BF16), ("u", [128, D], F32), ("v", [128, D], F32), ("vn", [128, D], BF16),
            ("mix", [128, D], F32), ("gated", [128, D], BF16), ("xn", [128, D], BF16), ("w0", [128, KC, 128], BF16), ("w2", [128, KC, 128], BF16))}
    with contextlib.ExitStack() as st0:
        S = Sched(nc, st0)
        T = {}
        sb0 = lambda n, sh, dt: st0.enter_context(nc.sbuf_tensor(_uq("sb_" + n), sh, dt))
        vfm = sb0("vfm", [128, NVF, KC], F32)
        for nm in ("mix_l", "mix_c", "ffn_l", "ffn_c"):
            T["A_" + nm] = sb0("A_" + nm, [128, KC], F32)
        T["ident_b"] = sb0("ident_b", [128, 128], BF16)
        x_t = sb0("x_t", [128, D], F32)
        T["sq"] = sb0("sq", [128, D], F32)
        T["ss"] = sb0("ss", [128, 1], F32)
        T["rstd"] = sb0("rstd", [128, 1], F32)
        gT = sb0("gT", [128, KC, 128], BF16)
        S.dma("sp", vfm[:], vfm_d, writes=["vec"])
        S.dma("sp", T["ident_b"][:], identb_d, writes=["ident_b"])
        mv = emit_modvecs(S, T, vfm)
        with contextlib.ExitStack() as st:
            sb = lambda n, sh, dt: st.enter_context(nc.sbuf_tensor(_uq("sb_" + n), sh, dt))
            ps = lambda n, sh, dt: st.enter_context(nc.psum_tensor(_uq("ps_" + n), sh, dt))
            win = sb("win", [128, KC, 2 * D], BF16)
            wsT = sb("wsT_s", [128, 8, 128], BF16)
            bs = sb("bs_s", [128, 8], F32)
            lng = sb("lng", [128, D], F32)
            xn = sb("xn", [128, D], BF16)
            hT = sb("hT_s", [128, KC, 128], BF16)
            u = sb("u", [128, D], F32)
            v = sb("v", [128, D], F32)
            vn = sb("vn", [128, D], BF16)
            gated = sb("gated", [128, D], BF16)
            mean = sb("mean", [128, 1], F32)
            pT = ps("pT", [128, KC, 128], BF16)
            pU = [ps(f"pU{i}", [128, 512], F32) for i in range(2)]
            pS = ps("pS", [128, D], F32)
            for c4 in range(4):
                S.dma("pool", win[:, :, c4 * 1024:(c4 + 1) * 1024], w_in[:, :, c4 * 1024:(c4 + 1) * 1024], writes=["win"])
            S.dma("pool", wsT[:], wsT_d, writes=["wsT"])
            if dbg:
                S.dma("sp", gts[1, :, 0, :], win[:, 0, 0:128], reads=["win"], writes=["win_chk"])
            S.dma("sp", bs[:], bs_d, writes=["bs"])
            S.dma("sp", lng[:], vtm_d[2:3, :].to_broadcast([128, D]), writes=["lng"])
            sq = T["sq"]
            for t in range(NTILE):
                lat = t < SEQ // 128
                A1, B1 = mv["mix_l"] if lat else mv["mix_c"]
                S.dma("sp", x_t[:], xa[t * 128:(t + 1) * 128, :], writes=["x_t"])
                emit_rstd(S, x_t[:], "x_t", sq[:], T["ss"][:], T["rstd"][:], D)
                S.op("dve", lambda e: e.tensor_scalar(out=xn[:], in0=x_t[:], scalar1=T["rstd"][:, 0:1], scalar2=None, op0=ALU.mult),
                     reads=["x_t", "rstd"], writes=["xn"])
                S.group("pe", [(lambda e, k=k: e.transpose(out=pT[:, k, :], in_=xn[:, k * 128:(k + 1) * 128], identity=T["ident_b"][:]))
                               for k in range(KC)], reads=["xn", "ident_b"], writes=["pT"])
                S.op("dve", lambda e: e.tensor_tensor(out=sq[:].rearrange("p (k n) -> p k n", k=KC), in0=pT[:], in1=_b3(A1, 128), op=ALU.mult),
                     reads=["pT", "vec"], writes=["sq"])
                S.op("dve", lambda e: e.tensor_tensor(out=hT[:], in0=sq[:].rearrange("p (k n) -> p k n", k=KC), in1=_b3(B1, 128), op=ALU.add),
                     reads=["sq", "vec"], writes=["hT"])
                for j in range(8):
                    p = pU[j % 2]
                    S.group("pe", [(lambda e, k=k, p=p, j=j: e.matmul(p[:], hT[:, k, :], win[:, k, j * 512:(j + 1) * 512],
                                                                      start=(k == 0), stop=(k == KC - 1))) for k in range(KC)],
                            reads=["hT", "win", "win_chk"], writes=[f"pU{j % 2}"])
                    dst = u if j < 4 else v
                    dk = "u" if j < 4 else "v"
                    S.op("act", lambda e, p=p, dst=dst, j=j: e.activation(out=dst[:, (j % 4) * 512:(j % 4 + 1) * 512], in_=p[:], func=GELU_FUNC[0]),
                         reads=[f"pU{j % 2}"], writes=[dk])
                S.op("dve", lambda e: e.reduce_sum(out=mean[:], in_=v[:], axis=AX.X), reads=["v"], writes=["mean"])
                S.op("dve", lambda e: e.tensor_scalar(out=mean[:], in0=mean[:], scalar1=-1.0 / D, scalar2=None, op0=ALU.mult),
                     reads=["mean"], writes=["mean"])
                S.op("dve", lambda e: e.tensor_scalar(out=v[:], in0=v[:], scalar1=mean[:, 0:1], scalar2=None, op0=ALU.add),
                     reads=["v", "mean"], writes=["v"])
                if dbg and t == 0:
                    S.dma("sp", dbo["v"], v[:], reads=["v"], writes=["dbg"])
                emit_rstd(S, v[:], "v", sq[:], T["ss"][:], T["rstd"][:], D)
                S.op("dve", lambda e: e.tensor_scalar(out=vn[:], in0=v[:], scalar1=T["rstd"][:, 0:1], scalar2=None, op0=ALU.mult),
                     reads=["v", "rstd"], writes=["vn"])
                S.group("pe", [(lambda e, g=g: e.matmul(pS[:, g * 256:(g + 1) * 256], wsT[:, g, :], vn[:, g * 256:(g + 1) * 256],
                                                        start=True, stop=True)) for g in range(8)],
                        reads=["vn", "wsT"], writes=["pS"])
                S.op("dve", lambda e: e.tensor_tensor(out=sq[:], in0=pS[:], in1=lng[:], op=ALU.mult), reads=["pS", "lng"], writes=["sq"])
                S.op("dve", lambda e: e.tensor_tensor(out=sq[:].rearrange("p (g c) -> p g c", g=8), in0=sq[:].rearrange("p (g c) -> p g c", g=8),
                                                      in1=_b3(bs[:], 256), op=ALU.add), reads=["sq", "bs"], writes=["sq"])
                if dbg and t == 0:
                    S.dma("sp", dbo["mix"], sq[:], reads=["sq"], writes=["dbg"])
                S.op("dve", lambda e: e.tensor_tensor(out=gated[:], in0=sq[:], in1=u[:], op=ALU.mult), reads=["sq", "u"], writes=["gated"])
                S.group("pe", [(lambda e, k=k: e.transpose(out=pT[:, k, :], in_=gated[:, k * 128:(k + 1) * 128], identity=T["ident_b"][:]))
                               for k in range(KC)], reads=["gated", "ident_b"], writes=["pT"])
                if dbg and t == 0:
                    S.dma("sp", dbo["gated"], gated[:], reads=["gated"], writes=["dbg"])
                    S.dma("sp", dbo["vn"], vn[:], reads=["vn"], writes=["dbg"])
                    S.dma("sp", dbo["u"], u[:], reads=["u"], writes=["dbg"])
                    S.dma("sp", dbo["hT"], hT[:], reads=["hT"], writes=["dbg"])
                    S.dma("sp", dbo["xn"], xn[:], reads=["xn"], writes=["dbg"])
                    S.dma("sp", dbo["w0"], win[:, :, 0:128], reads=["win"], writes=["dbg"])
                    S.dma("sp", dbo["w2"], win[:, :, 2048:2176], reads=["win"], writes=["dbg"])
                S.op("act", lambda e: e.activation(out=gT[:], in_=pT[:], func=ACTF.Copy), reads=["pT"], writes=["gT"])
                S.dma("sp", gts[t], gT[:], reads=["gT"], writes=[f"gts{t}"])
        S.barrier()
        with contextlib.ExitStack() as st:
            sb = lambda n, sh, dt: st.enter_context(nc.sbuf_tensor(_uq("sb_" + n), sh, dt))
            ps = lambda n, sh, dt: st.enter_context(nc.psum_tensor(_uq("ps_" + n), sh, dt))
            wout = sb("wout", [128, KC, D], BF16)
            G = [sb(f"G{i}", [128, D], F32) for i in range(2)]
            x1 = sb("x1_s", [128, D], F32)
            alloc_ffn_prep(nc, st, S, T, wr_d, br_d, identf_d, vtm_d, {"l": (3, 4, 5), "c": (3, 6, 7)})
            pY = ps("pY", [128, D], F32)
            T["pT32"] = ps("pT32", [128, KC, 128], F32)
            T["pR"] = pY[:, 0:NEXP]
            for c2 in range(2):
                S.dma("pool", wout[:, :, c2 * 1024:(c2 + 1) * 1024], w_out[:, :, c2 * 1024:(c2 + 1) * 1024], writes=["wout"])
            for i in range(2):
                S.dma("sp", G[i][:], vtm_d[i:i + 1, :].to_broadcast([128, D]), writes=[f"G{i}"])
            for t in range(NTILE):
                lat = t < SEQ // 128
                A2, B2 = mv["ffn_l"] if lat else mv["ffn_c"]
                gi = 0 if lat else 1
                S.dma("sp", x_t[:], xa[t * 128:(t + 1) * 128, :], writes=["x_t"])
                S.dma("sp", gT[:], gts[t], reads=[f"gts{t}"], writes=["gT"])
                for j in range(4):
                    S.group("pe", [(lambda e, k=k, j=j: e.matmul(pY[:, j * 512:(j + 1) * 512], gT[:, k, :], wout[:, k, j * 512:(j + 1) * 512],
                                                                 start=(k == 0), stop=(k == KC - 1))) for k in range(KC)],
                            reads=["gT", "wout"], writes=["pR"])
                S.op("dve", lambda e, gi=gi: e.tensor_tensor(out=x1[:], in0=pY[:], in1=G[gi][:], op=ALU.mult), reads=["pR", f"G{gi}"], writes=["x1"])
                S.op("dve", lambda e: e.tensor_tensor(out=x1[:], in0=x1[:], in1=x_t[:], op=ALU.add), reads=["x1", "x_t"], writes=["x1"])
                S.dma("sp", x1_o[t * 128:(t + 1) * 128, :], x1[:], reads=["x1"], writes=["x1_o"])
                emit_ffn_prep(S, T, x1, "x1", A2, B2, t * 128, hT_o, gates_o, var="l" if lat else "c")
        S.finish()
    return nc


def _fm(vec):
    return np.ascontiguousarray(vec.reshape(KC, 128).T)


def _rows_fm(w):
    return np.ascontiguousarray(w.reshape(KC, 128, -1).transpose(1, 0, 2))


def gmlp_inputs(b, x, ctx, mod0, norm_mix_g, norm_ffn_g, gmlp_w_in, gmlp_norm_g, gmlp_w_s, gmlp_b_s, gmlp_w_out, w_router, b_router):
    ml, mc = mod0[b], mod0[8]
    vfm = np.stack([_fm(norm_mix_g), _fm(ml[0]), _fm(ml[1]), _fm(mc[0]), _fm(mc[1]),
                    _fm(norm_ffn_g), _fm(ml[3]), _fm(ml[4]), _fm(mc[3]), _fm(mc[4])], axis=1)
    return {
        "xa": np.ascontiguousarray(np.concatenate([x[b], ctx[b]], axis=0)),
        "w_in": _rows_fm(gmlp_w_in), "w_out": _rows_fm(gmlp_w_out),
        "wsT": np.ascontiguousarray(gmlp_w_s.transpose(2, 0, 1)),
        "vfm": np.ascontiguousarray(vfm.astype(np.float32)),
        "vtm": np.ascontiguousarray(np.stack([ml[2], mc[2], gmlp_norm_g, norm_ffn_g, ml[3], ml[4], mc[3], mc[4]]).astype(np.float32)),
        "bs": np.ascontiguousarray(gmlp_b_s.T),
        "wr": _rows_fm(w_router), "br": np.ascontiguousarray(b_router[None, :]),
        "identf": np.eye(128, dtype=np.float32), "identb": np.eye(128, dtype=np.float32).astype(ml_dtypes.bfloat16),
    }


MT = 1024


def build_moe(NT):
    nc = _new_nc()
    din = lambda n, sh, dt=F32: nc.dram_tensor(n, sh, dt, kind="ExternalInput").ap()
    hT_d = din("hT", [128, KC, NT], BF16)
    g_d = din("gts", [128, NT // 128, EPC])
    wg_d = din("wg", [EPC, KC, 128, KC, 128])
    wl_d = din("wl", [EPC, KC, 128, KC, 128])
    bgl_d = din("bgl", [128, EPC, 2, KC])
    wd_d = din("wd", [EPC, 4, 128, KC, 512])
    bd_d = din("bd", [EPC, D])
    y_o = nc.dram_tensor("ypart", [NT, D], F32, kind="ExternalOutput").ap()
    with contextlib.ExitStack() as st:
        S = Sched(nc, st)
        sb = lambda n, sh, dt: st.enter_context(nc.sbuf_tensor(_uq("sb_" + n), sh, dt))
        ps = lambda n, sh, dt: st.enter_context(nc.psum_tensor(_uq("ps_" + n), sh, dt))
        HT = sb("HT", [128, KC, MT], BF16)
        actT = sb("actT", [128, KC, MT], BF16)
        yacc = sb("yacc", [128, MT // 128, D], F32)
        wgl = [sb(f"wgl{i}", [128, 2, KC, 128], BF16) for i in range(2)]
        wdb = [sb(f"wdb{i}", [128, KC, 512], BF16) for i in range(2)]
        bd = sb("bd", [128, D], F32)
        gts = sb("gts", [128, NT // 128, EPC], F32)
        bgl = sb("bgl", [128, EPC, 2, KC], F32)
        glu = [sb(f"glu{i}", [128, 512], F32) for i in range(2)]
        sig = [sb(f"sig{i}", [128, 512], F32) for i in range(2)]
        lin = [sb(f"lin{i}", [128, 512], F32) for i in range(2)]
        tmp = [sb(f"tmp{i}", [128, 512], F32) for i in range(2)]
        pg = [ps(f"pg{i}", [128, 512], F32) for i in range(2)]
        pl = [ps(f"pl{i}", [128, 512], F32) for i in range(2)]
        po = [ps(f"po{i}", [128, 512], F32) for i in range(2)]
        S.dma("sp", gts[:], g_d, writes=["gts"])
        S.dma("sp", bgl[:], bgl_d, writes=["bgl"])
        ci = 0
        di = 0
        gi = 0
        oi = 0
        for s in range(NT // MT):
            S.dma("sp", HT[:], hT_d[:, :, s * MT:(s + 1) * MT], writes=["HT"])
            for e in range(EPC):
                S.dma("sp", bd[:], bd_d[e:e + 1, :].to_broadcast([128, D]), writes=["bd"])
                for j in range(KC):
                    w = wgl[ci % 2]
                    wk = f"wgl{ci % 2}"
                    ci += 1
                    S.dma("pool", w[:, 0, :, :], wg_d[e, j], writes=[wk])
                    S.dma("pool", w[:, 1, :, :], wl_d[e, j], writes=[wk])
                    for tg in range(MT // 512):
                        b = gi % 2
                        gi += 1
                        S.group("pe", [(lambda e_, k=k, w=w, b=b, tg=tg: e_.matmul(pg[b][:], w[:, 0, k, :], HT[:, k, tg * 512:(tg + 1) * 512],
                                                                                   start=(k == 0), stop=(k == KC - 1))) for k in range(KC)],
                                reads=[wk, "HT"], writes=[f"pg{b}"])
                        S.group("pe", [(lambda e_, k=k, w=w, b=b, tg=tg: e_.matmul(pl[b][:], w[:, 1, k, :], HT[:, k, tg * 512:(tg + 1) * 512],
                                                                                   start=(k == 0), stop=(k == KC - 1))) for k in range(KC)],
                                reads=[wk, "HT"], writes=[f"pl{b}"])
                        S.op("dve", lambda e_, b=b, e=e, j=j: e_.tensor_scalar(out=glu[b][:], in0=pg[b][:], scalar1=bgl[:, e, 0, j:j + 1], scalar2=LIMIT,
                                                                          op0=ALU.add, op1=ALU.min), reads=[f"pg{b}", "bgl"], writes=[f"glu{b}"])
                        S.op("act", lambda e_, b=b: e_.activation(out=sig[b][:], in_=glu[b][:], func=ACTF.Sigmoid, scale=ALPHA),
                             reads=[f"glu{b}"], writes=[f"sig{b}"])
                        S.op("dve", lambda e_, b=b, e=e, j=j: e_.tensor_scalar(out=lin[b][:], in0=pl[b][:], scalar1=bgl[:, e, 1, j:j + 1], scalar2=LIMIT,
                                                                          op0=ALU.add, op1=ALU.min), reads=[f"pl{b}", "bgl"], writes=[f"lin{b}"])
                        S.op("dve", lambda e_, b=b: e_.tensor_scalar(out=lin[b][:], in0=lin[b][:], scalar1=-LIMIT, scalar2=1.0,
                                                                     op0=ALU.max, op1=ALU.add), reads=[f"lin{b}"], writes=[f"lin{b}"])
                        S.op("dve", lambda e_, b=b: e_.tensor_tensor(out=glu[b][:], in0=glu[b][:], in1=sig[b][:], op=ALU.mult),
                             reads=[f"glu{b}", f"sig{b}"], writes=[f"glu{b}"])
                        S.op("dve", lambda e_, b=b, j=j, tg=tg: e_.tensor_tensor(out=actT[:, j, tg * 512:(tg + 1) * 512], in0=glu[b][:], in1=lin[b][:], op=ALU.mult),
                             reads=[f"glu{b}", f"lin{b}"], writes=["actT"])
                for dg in range(4):
                    wd = wdb[di % 2]
                    dk = f"wdb{di % 2}"
                    di += 1
                    S.dma("pool", wd[:], wd_d[e, dg], writes=[dk])
                    for t in range(MT // 128):
                        b = oi % 2
                        oi += 1
                        S.group("pe", [(lambda e_, k=k, wd=wd, b=b, t=t: e_.matmul(po[b][:], actT[:, k, t * 128:(t + 1) * 128], wd[:, k, :],
                                                                                   start=(k == 0), stop=(k == KC - 1))) for k in range(KC)],
                                reads=["actT", dk], writes=[f"po{b}"])
                        S.op("dve", lambda e_, b=b, dg=dg: e_.tensor_tensor(out=tmp[b][:], in0=po[b][:], in1=bd[:, dg * 512:(dg + 1) * 512], op=ALU.add),
                             reads=[f"po{b}", "bd"], writes=[f"tmp{b}"])
                        gcol = gts[:, s * (MT // 128) + t, e:e + 1]
                        ysl = yacc[:, t, dg * 512:(dg + 1) * 512]
                        if e == 0:
                            S.op("dve", lambda e_, b=b, gcol=gcol, ysl=ysl: e_.tensor_scalar(out=ysl, in0=tmp[b][:], scalar1=gcol, scalar2=None, op0=ALU.mult),
                                 reads=[f"tmp{b}", "gts"], writes=["yacc"])
                        else:
                            S.op("dve", lambda e_, b=b, gcol=gcol, ysl=ysl: e_.scalar_tensor_tensor(out=ysl, in0=tmp[b][:], scalar=gcol, in1=ysl,
                                                                                                op0=ALU.mult, op1=ALU.add),
                                 reads=[f"tmp{b}", "gts", "yacc"], writes=["yacc"])
            S.dma("sp", y_o[s * MT:(s + 1) * MT, :].rearrange("(t p) d -> p t d", p=128), yacc[:], reads=["yacc"], writes=["y_o"])
        S.finish()
    return nc


def moe_weights(core, w_gu, b_gu, w_down, b_down):
    es = slice(core * EPC, (core + 1) * EPC)
    wgu = w_gu[es].reshape(EPC, KC, 128, KC, 128, 2)
    wg = np.ascontiguousarray(wgu[..., 0].transpose(0, 3, 2, 1, 4))
    wl = np.ascontiguousarray(wgu[..., 1].transpose(0, 3, 2, 1, 4))
    bgu = b_gu[es].reshape(EPC, KC, 128, 2)
    bgl = np.ascontiguousarray(bgu.transpose(2, 0, 3, 1))
    wdf = np.ascontiguousarray(w_down[es].reshape(EPC, KC, 128, D).transpose(0, 2, 1, 3))
    return {"wg": wg, "wl": wl, "bgl": bgl, "wdf": wdf, "bd": np.ascontiguousarray(b_down[es])}


def moe_gates(core, gates_all):
    NT = gates_all.shape[0]
    g = gates_all[:, core * EPC:(core + 1) * EPC].reshape(NT // 128, 128, EPC)
    return np.ascontiguousarray(g.transpose(1, 0, 2))


def emit_combine(S, x_t, xk, yp_t, ysum, gate_tile, gk, ypk="yp_t"):
    S.op("dve", lambda e: e.reduce_sum(out=ysum[:], in_=yp_t[:].rearrange("p c d -> p d c"), axis=AX.X), reads=[ypk], writes=["ysum"])
    S.op("dve", lambda e: e.tensor_tensor(out=ysum[:], in0=ysum[:], in1=gate_tile[:], op=ALU.mult), reads=["ysum", gk], writes=["ysum"])
    S.op("dve", lambda e: e.tensor_tensor(out=x_t[:], in0=x_t[:], in1=ysum[:], op=ALU.add), reads=["ysum", xk], writes=[xk])


def build_final():
    nc = _new_nc()
    din = lambda n, sh, dt=F32: nc.dram_tensor(n, sh, dt, kind="ExternalInput").ap()
    x3 = din("x3", [SEQ, D])
    yp = din("yp", [NCORE, SEQ, D])
    g_d = din("g5", [1, D])
    out = nc.dram_tensor("out", [SEQ, D], F32, kind="ExternalOutput").ap()
    with contextlib.ExitStack() as st:
        S = Sched(nc, st)
        sb = lambda n, sh, dt: st.enter_context(nc.sbuf_tensor(_uq("sb_" + n), sh, dt))
        x_t = [sb(f"x_t{i}", [128, D], F32) for i in range(2)]
        yp_b = [sb(f"yp_t{i}", [128, NCORE, D], F32) for i in range(2)]
        ysum = sb("ysum", [128, D], F32)
        G = sb("G", [128, D], F32)
        S.dma("sp", G[:], g_d.to_broadcast([128, D]), writes=["G"])
        for t in range(SEQ // 128):
            xt = x_t[t % 2]
            xk = f"x_t{t % 2}"
            S.dma("sp", xt[:], x3[t * 128:(t + 1) * 128, :], writes=[xk])
            yp_t, ypk = yp_b[t % 2], f"yp_t{t % 2}"
            S.dma("sp", yp_t[:], yp[:, t * 128:(t + 1) * 128, :].rearrange("c p d -> p c d"), writes=[ypk])
            emit_combine(S, xt, xk, yp_t, ysum, G, "G", ypk)
            S.dma("sp", out[t * 128:(t + 1) * 128, :], xt[:], reads=[xk], writes=["out"])
        S.finish()
    return nc


T_G5L, T_G5C, T_G2L, T_QG, T_KG, T_SG = range(6)
SCALE = HD ** -0.5


def build_attn(lam_init):
    nc = _new_nc()
    din = lambda n, sh, dt=F32: nc.dram_tensor(n, sh, dt, kind="ExternalInput").ap()
    x1_d = din("x1", [NTOK, D])
    yp_d = din("yp", [NCORE, NTOK, D])
    vtm_d = din("vtm", [9, D])
    vfm_d = din("vfm", [128, NVF, KC])
    wqkv_d = din("wqkv", [128, KC, 3 * D])
    wo_d = din("wo", [128, KC, D])
    cs_d = din("cs", [SEQ, 2, 2, 32])
    lam_d = din("lam4", [1, 4, HD])
    wr_d = din("wr", [128, KC, NEXP])
    br_d = din("br", [1, NEXP])
    identf_d = din("identf", [128, 128])
    identb_d = din("identb", [128, 128], BF16)
    x3_o = nc.dram_tensor("x3", [SEQ, D], F32, kind="ExternalOutput").ap()
    hT_o = nc.dram_tensor("htm", [SEQ, D], BF16, kind="ExternalOutput").ap()
    gates_o = nc.dram_tensor("gates", [SEQ, NEXP], F32, kind="ExternalOutput").ap()
    x2s = nc.dram_tensor("x2s", [NTOK, D], F32).ap()
    hTs = nc.dram_tensor("hTs", [NTILE, 128, KC, 128], BF16).ap()
    qTs = nc.dram_tensor("qTs", [SEQ // 128, 128, NH2, 128], BF16).ap()
    kTs = nc.dram_tensor("kTs", [NTILE, 128, NH2, 128], BF16).ap()
    vs = nc.dram_tensor("vs", [NTILE, 128, D], BF16).ap()
    osd = nc.dram_tensor("osd", [SEQ // 128, 128, D], BF16).ap()
    NLT = SEQ // 128
    with contextlib.ExitStack() as st0:
        S = Sched(nc, st0)
        T = {}
        sb0 = lambda n, sh, dt: st0.enter_context(nc.sbuf_tensor(_uq("sb_" + n), sh, dt))
        vfm = sb0("vfm", [128, NVF, KC], F32)
        for nm in ("mix_l", "mix_c", "ffn_l", "ffn_c"):
            T["A_" + nm] = sb0("A_" + nm, [128, KC], F32)
        T["ident_b"] = sb0("ident_b", [128, 128], BF16)
        T["sq"] = sb0("sq", [128, D], F32)
        T["ss"] = sb0("ss", [128, 1], F32)
        T["rstd"] = sb0("rstd", [128, 1], F32)
        S.dma("sp", vfm[:], vfm_d, writes=["vec"])
        S.dma("sp", T["ident_b"][:], identb_d, writes=["ident_b"])
        mv = emit_modvecs(S, T, vfm)
        sq = T["sq"]
        with contextlib.ExitStack() as st:
            sb = lambda n, sh, dt: st.enter_context(nc.sbuf_tensor(_uq("sb_" + n), sh, dt))
            ps = lambda n, sh, dt: st.enter_context(nc.psum_tensor(_uq("ps_" + n), sh, dt))
            x_t = sb("x_t", [128, D], F32)
            hT = sb("hT_s", [128, KC, 128], BF16)
            yp_b = [sb(f"yp_t{i}", [128, NCORE, D], F32) for i in range(2)]
            ysum = sb("ysum", [128, D], F32)
            G5 = [sb(f"G5{i}", [128, D], F32) for i in range(2)]
            xn = sb("xn", [128, D], BF16)
            pT = ps("pT", [128, KC, 128], BF16)
            for i in range(2):
                S.dma("sp", G5[i][:], vtm_d[i:i + 1, :].to_broadcast([128, D]), writes=[f"G5{i}"])
            for t in range(NTILE):
                lat = t < NLT
                A1, B1 = mv["mix_l"] if lat else mv["mix_c"]
                gi = 0 if lat else 1
                S.dma("sp", x_t[:], x1_d[t * 128:(t + 1) * 128, :], writes=["x_t"])
                yp_t, ypk = yp_b[t % 2], f"yp_t{t % 2}"
                S.dma("sp", yp_t[:], yp_d[:, t * 128:(t + 1) * 128, :].rearrange("c p d -> p c d"), writes=[ypk])
                emit_combine(S, x_t, "x_t", yp_t, ysum, G5[gi], f"G5{gi}", ypk)
                S.dma("sp", x2s[t * 128:(t + 1) * 128, :], x_t[:], reads=["x_t"], writes=[f"x2s{t}"])
                emit_rstd(S, x_t[:], "x_t", sq[:], T["ss"][:], T["rstd"][:], D)
                S.op("dve", lambda e: e.tensor_scalar(out=xn[:], in0=x_t[:], scalar1=T["rstd"][:, 0:1], scalar2=None, op0=ALU.mult),
                     reads=["x_t", "rstd"], writes=["xn"])
                S.group("pe", [(lambda e, k=k: e.transpose(out=pT[:, k, :], in_=xn[:, k * 128:(k + 1) * 128], identity=T["ident_b"][:]))
                               for k in range(KC)], reads=["xn", "ident_b"], writes=["pT"])
                S.op("dve", lambda e: e.tensor_tensor(out=sq[:].rearrange("p (k n) -> p k n", k=KC), in0=pT[:], in1=_b3(A1, 128), op=ALU.mult),
                     reads=["pT", "vec"], writes=["sq"])
                S.op("dve", lambda e: e.tensor_tensor(out=hT[:], in0=sq[:].rearrange("p (k n) -> p k n", k=KC), in1=_b3(B1, 128), op=ALU.add),
                     reads=["sq", "vec"], writes=["hT"])
                S.dma("sp", hTs[t], hT[:], reads=["hT"], writes=[f"hTs{t}"])
        S.barrier()
        with contextlib.ExitStack() as st:
            sb = lambda n, sh, dt: st.enter_context(nc.sbuf_tensor(_uq("sb_" + n), sh, dt))
            ps = lambda n, sh, dt: st.enter_context(nc.psum_tensor(_uq("ps_" + n), sh, dt))
            wp = sb("wp", [128, KC, D], BF16)
            hT = sb("hT_s", [128, KC, 128], BF16)
            QG = sb("QG", [128, D], F32)
            KG = sb("KG", [128, D], F32)
            qn = sb("qn", [128, D], F32)
            qr = sb("qr", [128, D], BF16)
            qT = sb("qT", [128, NH2, 128], BF16)
            cs = sb("cs", [128, 2, 2, 32], F32)
            ta = sb("ta", [128, NH2 * 2 * 32], F32)
            tb = sb("tb", [128, NH2 * 2 * 32], F32)
            ss16 = sb("ss16", [128, NH2], F32)
            pQ = ps("pQ", [128, D], F32)
            pT = ps("pT", [128, NH2, 128], BF16)
            S.dma("sp", QG[:], vtm_d[T_QG:T_QG + 1, :].to_broadcast([128, D]), writes=["QG"])
            S.dma("sp", KG[:], vtm_d[T_KG:T_KG + 1, :].to_broadcast([128, D]), writes=["KG"])
            for part in range(3):
                for c2 in range(2):
                    S.dma("pool", wp[:, :, c2 * 1024:(c2 + 1) * 1024], wqkv_d[:, :, part * D + c2 * 1024:part * D + (c2 + 1) * 1024],
                          writes=["wp"])
                tiles = range(NLT) if part == 0 else range(NTILE)
                for t in tiles:
                    lat = t < NLT
                    S.dma("sp", hT[:], hTs[t], reads=[f"hTs{t}"], writes=["hT"])
                    for j in range(4):
                        S.group("pe", [(lambda e, k=k, j=j: e.matmul(pQ[:, j * 512:(j + 1) * 512], hT[:, k, :], wp[:, k, j * 512:(j + 1) * 512],
                                                                     start=(k == 0), stop=(k == KC - 1))) for k in range(KC)],
                                reads=["hT", "wp"], writes=["pQ"])
                    if part == 2:
                        S.op("act", lambda e: e.activation(out=qr[:], in_=pQ[:], func=ACTF.Copy), reads=["pQ"], writes=["qr"])
                        S.dma("sp", vs[t], qr[:], reads=["qr"], writes=[f"vs{t}"])
                        continue
                    GT, gk = (QG, "QG") if part == 0 else (KG, "KG")
                    S.op("act", lambda e: e.activation(out=sq[:], in_=pQ[:], func=ACTF.Square), reads=["pQ"], writes=["sq"])
                    S.op("dve", lambda e: e.reduce_sum(out=ss16[:], in_=sq[:].rearrange("p (h d) -> p h d", h=NH2), axis=AX.X),
                         reads=["sq"], writes=["ss16"])
                    S.op("act", lambda e: e.activation(out=ss16[:], in_=ss16[:], func=ACTF.Sqrt, scale=1.0 / HD, bias=EPS),
                         reads=["ss16"], writes=["ss16"])
                    S.op("dve", lambda e: e.reciprocal(out=ss16[:], in_=ss16[:]), reads=["ss16"], writes=["ss16"])
                    S.op("dve", lambda e: e.tensor_tensor(out=qn[:].rearrange("p (h d) -> p h d", h=NH2), in0=pQ[:].rearrange("p (h d) -> p h d", h=NH2),
                                                          in1=_b3(ss16[:], HD), op=ALU.mult), reads=["pQ", "ss16"], writes=["qn"])
                    if lat:
                        S.op("dve", lambda e, GT=GT: e.tensor_tensor(out=qn[:], in0=qn[:], in1=GT[:], op=ALU.mult), reads=["qn", gk], writes=["qn"])
                        S.dma("sp", cs[:], cs_d[t * 128:(t + 1) * 128], writes=["cs"])
                        v5 = lambda ap: ap.rearrange("p (h a f r) -> p h a f r", h=NH2, a=2, f=2, r=32)
                        x1v, x2v = v5(qn[:])[:, :, :, 0, :], v5(qn[:])[:, :, :, 1, :]
                        o1v, o2v = v5(qr[:])[:, :, :, 0, :], v5(qr[:])[:, :, :, 1, :]
                        cosb = cs[:, 0, :, :].unsqueeze(1).to_broadcast([128, NH2, 2, 32])
                        sinb = cs[:, 1, :, :].unsqueeze(1).to_broadcast([128, NH2, 2, 32])
                        tav = ta[:].rearrange("p (h a r) -> p h a r", h=NH2, a=2)
                        tbv = tb[:].rearrange("p (h a r) -> p h a r", h=NH2, a=2)
                        S.op("dve", lambda e: e.tensor_tensor(out=tav, in0=x1v, in1=cosb, op=ALU.mult), reads=["qn", "cs"], writes=["ta"])
                        S.op("pool", lambda e: e.tensor_tensor(out=tbv, in0=x2v, in1=sinb, op=ALU.mult), reads=["qn", "cs"], writes=["tb"])
                        S.op("dve", lambda e: e.tensor_tensor(out=o1v, in0=tav, in1=tbv, op=ALU.subtract), reads=["ta", "tb"], writes=["qr"])
                        S.op("dve", lambda e: e.tensor_tensor(out=tav, in0=x2v, in1=cosb, op=ALU.mult), reads=["qn", "cs"], writes=["ta"])
                        S.op("pool", lambda e: e.tensor_tensor(out=tbv, in0=x1v, in1=sinb, op=ALU.mult), reads=["qn", "cs"], writes=["tb"])
                        S.op("dve", lambda e: e.tensor_tensor(out=o2v, in0=tav, in1=tbv, op=ALU.add), reads=["ta", "tb"], writes=["qr"])
                    else:
                        S.op("dve", lambda e, GT=GT: e.tensor_tensor(out=qr[:], in0=qn[:], in1=GT[:], op=ALU.mult), reads=["qn", gk], writes=["qr"])
                    S.group("pe", [(lambda e, h=h: e.transpose(out=pT[:, h, :], in_=qr[:, h * 128:(h + 1) * 128], identity=T["ident_b"][:]))
                                   for h in range(NH2)], reads=["qr", "ident_b"], writes=["pT"])
                    S.op("act", lambda e: e.activation(out=qT[:], in_=pT[:], func=ACTF.Copy), reads=["pT"], writes=["qT"])
                    if part == 0:
                        S.dma("sp", qTs[t], qT[:], reads=["qT"], writes=[f"qTs{t}"])
                    else:
                        S.dma("sp", kTs[t], qT[:], reads=["qT"], writes=[f"kTs{t}"])
        S.barrier()
        with contextlib.ExitStack() as st:
            sb = lambda n, sh, dt: st.enter_context(nc.sbuf_tensor(_uq("sb_" + n), sh, dt))
            ps = lambda n, sh, dt: st.enter_context(nc.psum_tensor(_uq("ps_" + n), sh, dt))
            kT = sb("kT", [128, NH2, NTOK], BF16)
            va = sb("va", [128, NTILE, D], BF16)
            qTg = sb("qTg", [128, NH2, 512], BF16)
            PT = [sb(f"PT{i}", [128, 512], BF16) for i in range(2)]
            o_sb = sb("o_sb", [128, 4, D], BF16)
            o4 = sb("o4", [128, 4, 256], F32)
            o4b = sb("o4b", [128, 4, 256], F32)
            ss4 = sb("ss4", [128, 4], F32)
            rl4 = sb("rl4", [128, 4], F32)
            ones = sb("ones", [128, 1], BF16)
            onesf = sb("onesf", [1, 128], F32)
            SG = sb("SG", [128, D], F32)
            lam4 = sb("lam4", [1, 4, HD], F32)
            lt = sb("lt", [1, 2, HD], F32)
            l2 = sb("l2", [1, 2], F32)
            nlam = sb("nlam", [128, 1], F32)
            rs = sb("rs", [128, 8], F32)
            rl = sb("rl", [128, 1], F32)
            pS = [ps(f"pS{i}", [128, 512], F32) for i in range(2)]
            pO = ps("pO", [128, 8, 256], F32)
            pSm = ps("pSm", [128, 8], F32)
            for t in range(NTILE):
                S.dma("sp", kT[:, :, t * 128:(t + 1) * 128], kTs[t], reads=[f"kTs{t}"], writes=["kT"])
                S.dma("sp", va[:, t, :], vs[t], reads=[f"vs{t}"], writes=["va"])
            S.dma("sp", SG[:], vtm_d[T_SG:T_SG + 1, :].to_broadcast([128, D]), writes=["SG"])
            S.dma("sp", lam4[:], lam_d, writes=["lam4"])
            S.op("dve", lambda e: e.memset(ones[:], 1.0), writes=["ones"])
            S.op("dve", lambda e: e.memset(onesf[:], 1.0), writes=["onesf"])
            S.op("dve", lambda e: e.tensor_scalar(out=SG[:], in0=SG[:], scalar1=float(1.0 - lam_init), scalar2=None, op0=ALU.mult), reads=["SG"], writes=["SG"])
            S.op("dve", lambda e: e.tensor_tensor(out=lt[:, 0, :], in0=lam4[:, 0, :], in1=lam4[:, 1, :], op=ALU.mult), reads=["lam4"], writes=["lt"])
            S.op("dve", lambda e: e.tensor_tensor(out=lt[:, 1, :], in0=lam4[:, 2, :], in1=lam4[:, 3, :], op=ALU.mult), reads=["lam4", "lt"], writes=["lt"])
            S.op("dve", lambda e: e.reduce_sum(out=l2[:], in_=lt[:], axis=AX.X), reads=["lt"], writes=["l2"])
            S.op("act", lambda e: e.activation(out=l2[:], in_=l2[:], func=ACTF.Exp), reads=["l2"], writes=["l2"])
            S.op("dve", lambda e: e.tensor_tensor(out=l2[:, 0:1], in0=l2[:, 1:2], in1=l2[:, 0:1], op=ALU.subtract), reads=["l2"], writes=["l2"])
            S.op("dve", lambda e: e.tensor_scalar(out=l2[:, 0:1], in0=l2[:, 0:1], scalar1=-float(lam_init), scalar2=None, op0=ALU.add), reads=["l2"], writes=["l2"])
            S.op("pe", lambda e: e.matmul(pSm[:, 0:1], onesf[:], l2[:, 0:1], start=True, stop=True), reads=["onesf", "l2"], writes=["pSm"])
            S.op("dve", lambda e: e.tensor_copy(out=nlam[:], in_=pSm[:, 0:1]), reads=["pSm"], writes=["nlam"])
            si = 0
            for qg in range(4):
                for i in range(4):
                    S.dma("sp", qTg[:, :, i * 128:(i + 1) * 128], qTs[qg * 4 + i], reads=[f"qTs{qg * 4 + i}"], writes=["qTg"])
                for h in range(NH):
                    for j in range(2):
                        hh = 2 * h + j
                        for kc in range(NTILE):
                            b = si % 2
                            si += 1
                            S.op("pe", lambda e, b=b, hh=hh, kc=kc: e.matmul(pS[b][:], kT[:, hh, kc * 128:(kc + 1) * 128], qTg[:, hh, :], start=True, stop=True),
                                 reads=["kT", "qTg"], writes=[f"pS{b}"])
                            S.op("act", lambda e, b=b: e.activation(out=PT[b][:], in_=pS[b][:], func=ACTF.Exp, scale=SCALE),
                                 reads=[f"pS{b}"], writes=[f"PT{b}"])
                            fns = []
                            for qt in range(4):
                                fns.append(lambda e, b=b, qt=qt, j=j, kc=kc, h=h: e.matmul(pO[:, j * 4 + qt, :], PT[b][:, qt * 128:(qt + 1) * 128],
                                                                                         va[:, kc, h * 256:(h + 1) * 256], start=(kc == 0), stop=(kc == NTILE - 1)))
                                fns.append(lambda e, b=b, qt=qt, j=j, kc=kc: e.matmul(pSm[:, j * 4 + qt:j * 4 + qt + 1], PT[b][:, qt * 128:(qt + 1) * 128],
                                                                                    ones[:], start=(kc == 0), stop=(kc == NTILE - 1)))
                            S.group("pe", fns, reads=[f"PT{b}", "va", "ones"], writes=["pO", "pSm"])
                    S.op("dve", lambda e: e.reciprocal(out=rs[:], in_=pSm[:]), reads=["pSm"], writes=["rs"])
                    S.op("dve", lambda e: e.tensor_scalar(out=rl4[:], in0=rs[:, 4:8], scalar1=nlam[:, 0:1], scalar2=None, op0=ALU.mult),
                         reads=["rs", "nlam"], writes=["rl4"])
                    S.op("dve", lambda e: e.tensor_tensor(out=o4[:], in0=pO[:, 0:4, :], in1=_b3(rs[:, 0:4], 256), op=ALU.mult),
                         reads=["pO", "rs"], writes=["o4"])
                    S.op("dve", lambda e: e.tensor_tensor(out=o4b[:], in0=pO[:, 4:8, :], in1=_b3(rl4[:], 256), op=ALU.mult),
                         reads=["pO", "rl4"], writes=["o4b"])
                    S.op("dve", lambda e: e.tensor_tensor(out=o4[:], in0=o4[:], in1=o4b[:], op=ALU.add), reads=["o4", "o4b"], writes=["o4"])
                    S.op("act", lambda e: e.activation(out=o4b[:], in_=o4[:], func=ACTF.Square), reads=["o4"], writes=["o4b"])
                    S.op("dve", lambda e: e.reduce_sum(out=ss4[:], in_=o4b[:], axis=AX.X), reads=["o4b"], writes=["ss4"])
                    S.op("act", lambda e: e.activation(out=ss4[:], in_=ss4[:], func=ACTF.Sqrt, scale=1.0 / 256, bias=EPS), reads=["ss4"], writes=["ss4"])
                    S.op("dve", lambda e: e.reciprocal(out=ss4[:], in_=ss4[:]), reads=["ss4"], writes=["ss4"])
                    S.op("dve", lambda e: e.tensor_tensor(out=o4[:], in0=o4[:], in1=_b3(ss4[:], 256), op=ALU.mult), reads=["o4", "ss4"], writes=["o4"])
                    S.op("dve", lambda e, h=h: e.tensor_tensor(out=o_sb[:, :, h * 256:(h + 1) * 256], in0=o4[:],
                                                               in1=SG[:, h * 256:(h + 1) * 256].unsqueeze(1).to_broadcast([128, 4, 256]), op=ALU.mult),
                         reads=["o4", "SG"], writes=["o_sb"])
                for i in range(4):
                    S.dma("sp", osd[qg * 4 + i], o_sb[:, i, :], reads=["o_sb"], writes=[f"osd{qg * 4 + i}"])
        S.barrier()
        with contextlib.ExitStack() as st:
            sb = lambda n, sh, dt: st.enter_context(nc.sbuf_tensor(_uq("sb_" + n), sh, dt))
            ps = lambda n, sh, dt: st.enter_context(nc.psum_tensor(_uq("ps_" + n), sh, dt))
            wo = sb("wo", [128, KC, D], BF16)
            x_t = sb("x_t", [128, D], F32)
            G2 = sb("G2", [128, D], F32)
            o_t = sb("o_t", [128, D], BF16)
            oT = sb("oT", [128, KC, 128], BF16)
            x3 = sb("x3_s", [128, D], F32)
            alloc_ffn_prep(nc, st, S, T, wr_d, br_d, identf_d, vtm_d, {"l": (6, 7, 8)})
            pY = ps("pY", [128, D], F32)
            T["pT32"] = ps("pT32", [128, KC, 128], F32)
            T["pR"] = pY[:, 0:NEXP]
            pTb = T["pT32"][:].bitcast(BF16)[:, 0:KC, 0:128] if False else None
            for c2 in range(2):
                S.dma("pool", wo[:, :, c2 * 1024:(c2 + 1) * 1024], wo_d[:, :, c2 * 1024:(c2 + 1) * 1024], writes=["wo"])
            S.dma("sp", G2[:], vtm_d[T_G2L:T_G2L + 1, :].to_broadcast([128, D]), writes=["G2"])
            A2, B2 = mv["ffn_l"]
            for t in range(NLT):
                S.dma("sp", o_t[:], osd[t], reads=[f"osd{t}"], writes=["o_t"])
                S.dma("sp", x_t[:], x2s[t * 128:(t + 1) * 128, :], reads=[f"x2s{t}"], writes=["x_t"])
                pYb = pY[:].bitcast(BF16).rearrange("p (k n) -> p k n", n=128)
                S.group("pe", [(lambda e, k=k: e.transpose(out=pYb[:, k, :], in_=o_t[:, k * 128:(k + 1) * 128], identity=T["ident_b"][:]))
                               for k in range(KC)], reads=["o_t", "ident_b"], writes=["pR"])
                S.op("act", lambda e: e.activation(out=oT[:], in_=pYb[:, 0:KC, :], func=ACTF.Copy), reads=["pR"], writes=["oT"])
                for j in range(4):
                    S.group("pe", [(lambda e, k=k, j=j: e.matmul(pY[:, j * 512:(j + 1) * 512], oT[:, k, :], wo[:, k, j * 512:(j + 1) * 512],
                                                                 start=(k == 0), stop=(k == KC - 1))) for k in range(KC)],
                            reads=["oT", "wo"], writes=["pR"])
                S.op("dve", lambda e: e.tensor_tensor(out=x3[:], in0=pY[:], in1=G2[:], op=ALU.mult), reads=["pR", "G2"], writes=["x3"])
                S.op("dve", lambda e: e.tensor_tensor(out=x3[:], in0=x3[:], in1=x_t[:], op=ALU.add), reads=["x3", "x_t"], writes=["x3"])
                S.dma("sp", x3_o[t * 128:(t + 1) * 128, :], x3[:], reads=["x3"], writes=["x3_o"])
                emit_ffn_prep(S, T, x3, "x3", A2, B2, t * 128, hT_o, gates_o)
        S.finish()
    return nc


def rope_tables():
    rows = SEQ // 64
    row_pos = np.repeat(np.arange(rows), 64).astype(np.float32)
    col_pos = np.tile(np.arange(64), rows).astype(np.float32)
    inv = (10000.0 ** (-np.arange(32, dtype=np.float32) / 32)).astype(np.float32)
    ang = np.stack([row_pos[:, None] * inv, col_pos[:, None] * inv], axis=1)
    return np.ascontiguousarray(np.stack([np.cos(ang), np.sin(ang)], axis=1).astype(np.float32))


def attn_inputs(b, x1, yp, mod0, mod1, norm_mix_g, norm_ffn_g, w_qkv, q_g, k_g, lam4, subln_g, w_o, w_router, b_router, cs):
    ml0, mc0 = mod0[b], mod0[8]
    ml, mc = mod1[b], mod1[8]
    vfm = np.stack([_fm(norm_mix_g), _fm(ml[0]), _fm(ml[1]), _fm(mc[0]), _fm(mc[1]),
                    _fm(norm_ffn_g), _fm(ml[3]), _fm(ml[4]), _fm(mc[3]), _fm(mc[4])], axis=1)
    vtm = np.stack([ml0[5], mc0[5], ml[2], np.tile(q_g, NH2), np.tile(k_g, NH2), np.tile(subln_g, NH),
                    norm_ffn_g, ml[3], ml[4]]).astype(np.float32)
    return {
        "x1": x1, "yp": yp, "vtm": np.ascontiguousarray(vtm), "vfm": np.ascontiguousarray(vfm.astype(np.float32)),
        "wqkv": _rows_fm(w_qkv), "wo": _rows_fm(w_o), "cs": cs, "lam4": np.ascontiguousarray(lam4[None]),
        "wr": _rows_fm(w_router), "br": np.ascontiguousarray(b_router[None, :]),
        "identf": np.eye(128, dtype=np.float32), "identb": np.eye(128, dtype=np.float32).astype(ml_dtypes.bfloat16),
    }


def _run(nc, in_maps):
    res = run_bass_kernel_spmd(nc, in_maps, core_ids=list(range(NCORE)))
    return res.results


def _run_moe(NT, hT_list, gates_list, w_gu, b_gu, w_down, b_down):
    hT_all = np.ascontiguousarray(np.concatenate(hT_list, axis=2))
    gates_all = np.concatenate(gates_list, axis=0)
    ims = []
    for c in range(NCORE):
        m = moe_weights(c, w_gu, b_gu, w_down, b_down)
        m["hT"] = hT_all
        m["gts"] = moe_gates(c, gates_all)
        ims.append(m)
    res = _run(build_moe(NT), ims)
    return [np.asarray(r["ypart"]) for r in res]


def kernel(x, c, ctx, c_ctx, w_mod, b_mod, norm_mix_g, norm_ffn_g,
           gmlp_w_in, gmlp_norm_g, gmlp_w_s, gmlp_b_s, gmlp_w_out,
           diff_w_qkv, diff_q_norm_g, diff_k_norm_g, diff_lambda, diff_subln_g, diff_w_o,
           moe_w_router, moe_b_router, moe_w_gate_up, moe_b_gate_up, moe_w_down, moe_b_down):
    f = lambda a: np.asarray(a, dtype=np.float32)
    x, c, ctx, c_ctx, w_mod, b_mod = f(x), f(c), f(ctx), f(c_ctx), f(w_mod), f(b_mod)
    norm_mix_g, norm_ffn_g = f(norm_mix_g), f(norm_ffn_g)
    moe_w_gate_up, moe_b_gate_up, moe_w_down, moe_b_down = f(moe_w_gate_up), f(moe_b_gate_up), f(moe_w_down), f(moe_b_down)
    moe_w_router, moe_b_router = f(moe_w_router), f(moe_b_router)
    mod = run_mod(c, c_ctx, w_mod, b_mod)
    ims = [gmlp_inputs(b, x, ctx, mod[0], norm_mix_g[0], norm_ffn_g[0], f(gmlp_w_in)[0], f(gmlp_norm_g)[0], f(gmlp_w_s)[0],
                       f(gmlp_b_s)[0], f(gmlp_w_out)[0], moe_w_router[0], moe_b_router[0]) for b in range(NCORE)]
    resA = _run(build_gmlp(), ims)
    x1 = [np.asarray(r["x1"]) for r in resA]
    yp0 = _run_moe2(NCORE * NTOK, [np.asarray(r["htm"]) for r in resA], [np.asarray(r["gates"]) for r in resA],
                   moe_w_gate_up[0], moe_b_gate_up[0], moe_w_down[0], moe_b_down[0])
    del resA
    import math
    lam_init = 0.8 - 0.6 * math.exp(-0.3 * 1)
    cs = rope_tables()
    ims = []
    for b in range(NCORE):
        yp = np.ascontiguousarray(np.stack([yp0[cc][b * NTOK:(b + 1) * NTOK] for cc in range(NCORE)], axis=0))
        ims.append(attn_inputs(b, x1[b], yp, mod[0], mod[1], norm_mix_g[1], norm_ffn_g[1], f(diff_w_qkv)[0], f(diff_q_norm_g)[0],
                               f(diff_k_norm_g)[0], f(diff_lambda)[0], f(diff_subln_g)[0], f(diff_w_o)[0],
                               moe_w_router[1], moe_b_router[1], cs))
    del yp0
    resC = _run(build_attn(lam_init), ims)
    del ims
    x3 = [np.asarray(r["x3"]) for r in resC]
    yp1 = _run_moe2(NCORE * SEQ, [np.asarray(r["htm"]) for r in resC], [np.asarray(r["gates"]) for r in resC],
                   moe_w_gate_up[1], moe_b_gate_up[1], moe_w_down[1], moe_b_down[1])
    del resC
    ims = []
    for b in range(NCORE):
        yp = np.ascontiguousarray(np.stack([yp1[cc][b * SEQ:(b + 1) * SEQ] for cc in range(NCORE)], axis=0))
        ims.append({"x3": x3[b], "yp": yp, "g5": np.ascontiguousarray(mod[1][b][5][None, :])})
    del yp1
    resE = _run(build_final(), ims)
    return np.stack([np.asarray(r["out"]) for r in resE], axis=0).astype(np.float32)


I32 = mybir.dt.int32
CAP = 4096
MT2 = 2048
DW = 256
BIGIDX = 1.0e6


def _ind_dma(S, nc, reads, writes, **kw):
    S._deps("pool", reads, writes)
    lst = S.dma_sems["pool"]
    ent = lst[S.dma_rr["pool"] % len(lst)]
    S.dma_rr["pool"] += 1
    if ent[2] > 0:
        S._wait("pool", (ent[0], ent[1], ent[2], "dma"))
    ins = nc.gpsimd.indirect_dma_start(**kw)
    ent[2] += 16
    ins.then_inc(ent[0], 16)
    S._record((ent[0], ent[1], ent[2], "dma"), reads, writes)


def build_moe2(NT):
    F = NT // 128
    NB = CAP // 128
    NROW = NT + 128
    nc = _new_nc()
    din = lambda n, sh, dt=F32: nc.dram_tensor(n, sh, dt, kind="ExternalInput").ap()
    h_d = din("htm", [NROW, D], BF16)
    gpm_d = din("gpm", [128, EPC, F])
    gtm_d = din("gtm", [NROW, EPC])
    U_d = din("U", [128, 128])
    tid_d = din("tid", [128, F])
    pid_d = din("pid", [128, 1])
    identb_d = din("identb", [128, 128], BF16)
    wg_d = din("wg", [EPC, KC, 128, KC, 128])
    wl_d = din("wl", [EPC, KC, 128, KC, 128])
    bgl_d = din("bgl", [128, EPC, 2, KC])
    wd_d = din("wd8", [EPC, D // DW, 128, KC, DW])
    bd_d = din("bd", [EPC, D])
    y_o = [nc.dram_tensor(f"ypart{dg}", [NROW, DW], F32, kind="ExternalOutput").ap() for dg in range(D // DW)]
    lists = [nc.dram_tensor(f"lists{ex}", [CAP, 2], I32).ap() for ex in range(EPC)]
    with contextlib.ExitStack() as st:
        S = Sched(nc, st)
        sb = lambda n, sh, dt: st.enter_context(nc.sbuf_tensor(_uq("sb_" + n), sh, dt))
        ps = lambda n, sh, dt: st.enter_context(nc.psum_tensor(_uq("ps_" + n), sh, dt))
        XT = sb("XT", [128, KC, MT2], BF16)
        actT = sb("actT", [128, KC, MT2], BF16)
        wgl = [sb(f"wgl{i}", [128, 2, KC, 128], BF16) for i in range(2)]
        wdb = [sb(f"wdb{i}", [128, KC, DW], BF16) for i in range(2)]
        bd = sb("bd", [128, D], F32)
        bgl = sb("bgl", [128, EPC, 2, KC], F32)
        glu = [sb(f"glu{i}", [128, 512], F32) for i in range(2)]
        sig = [sb(f"sig{i}", [128, 512], F32) for i in range(2)]
        lin = [sb(f"lin{i}", [128, 512], F32) for i in range(2)]
        tmp = [sb(f"tmp{i}", [128, DW], F32) for i in range(4)]
        xg = [sb(f"xg{i}", [128, D], BF16) for i in range(2)]
        gsl = sb("gsl", [128, MT2 // 128, EPC], F32)
        identb = sb("identb", [128, 128], BF16)
        gpm = sb("gpm", [128, EPC, F], F32)
        U = sb("U", [128, 128], F32)
        tid = sb("tid", [128, F], F32)
        tid2 = sb("tid2", [128, F, 2], I32)
        pid = sb("pid", [128, 1], F32)
        m = sb("m", [128, F], F32)
        ca = sb("ca", [128, F], F32)
        cb = sb("cb", [128, F], F32)
        off = sb("off", [128, 1], F32)
        dest = sb("dest", [128, F], F32)
        desti = sb("desti", [128, F], I32)
        pref = sb("pref", [128, 1], F32)
        pre = sb("pre", [128, NB, 2], I32)
        lst = [sb(f"lst{e}", [128, NB, 2], I32) for e in range(EPC)]
        pT = ps("pT", [128, KC, 128], BF16)
        pg = [ps(f"pg{i}", [128, 512], F32) for i in range(2)]
        pl = [ps(f"pl{i}", [128, 512], F32) for i in range(2)]
        po = [ps(f"po{i}", [128, 512], F32) for i in range(2)]
        for n, t_, d_ in (("gpm", gpm, gpm_d), ("U", U, U_d), ("tid", tid, tid_d), ("pid", pid, pid_d), ("identb", identb, identb_d), ("bgl", bgl, bgl_d)):
            S.dma("sp", t_[:], d_, writes=[n])
        reg_cap = nc.gpsimd.alloc_register("bc_cap")
        nc.gpsimd.reg_mov(reg_cap, CAP - 1)
        reg_row = nc.gpsimd.alloc_register("bc_row")
        nc.gpsimd.reg_mov(reg_row, NROW - 1)
        S.op("dve", lambda e: e.memset(bd[:], 0.0), writes=["bd"])
        for dg in range(D // DW):
            for r in range(0, NROW, 1024):
                n = min(1024, NROW - r)
                S.dma("sp", y_o[dg][r:r + n, :].rearrange("(a p) d -> p a d", p=128), bd[:, 0:(n // 128) * DW].rearrange("p (a d) -> p a d", d=DW),
                      reads=["bd"], writes=["y0"])
        S.op("dve", lambda e: e.tensor_scalar(out=pref[:], in0=pid[:], scalar1=float(NT), scalar2=None, op0=ALU.add), reads=["pid"], writes=["pref"])
        S.op("dve", lambda e: e.tensor_copy(out=pre[:].rearrange("p b o -> p (b o)"), in_=pref[:, 0:1].to_broadcast([128, 2 * NB])), reads=["pref"], writes=["pre"])
        S.op("dve", lambda e: e.tensor_copy(out=tid2[:], in_=tid[:].unsqueeze(2).to_broadcast([128, F, 2])), reads=["tid"], writes=["tid2"])
        for ex in range(EPC):
            S.dma("sp", lists[ex].rearrange("(b p) o -> p b o", p=128), pre[:], reads=["pre"], writes=[f"list0_{ex}"])
            S.op("dve", lambda e, ex=ex: e.tensor_scalar(out=m[:], in0=gpm[:, ex, :], scalar1=0.0, scalar2=None, op0=ALU.is_gt), reads=["gpm"], writes=["m"])
            S.op("dve", lambda e: e.tensor_copy(out=ca[:], in_=m[:]), reads=["m"], writes=["ca"])
            cur, nxt, ck, nk = ca, cb, "ca", "cb"
            s_ = 1
            while s_ < F:
                S.op("dve", lambda e, cur=cur, nxt=nxt, s_=s_: e.tensor_copy(out=nxt[:, 0:s_], in_=cur[:, 0:s_]), reads=[ck], writes=[nk])
                S.op("dve", lambda e, cur=cur, nxt=nxt, s_=s_: e.tensor_tensor(out=nxt[:, s_:F], in0=cur[:, s_:F], in1=cur[:, 0:F - s_], op=ALU.add),
                     reads=[ck], writes=[nk])
                cur, nxt, ck, nk = nxt, cur, nk, ck
                s_ *= 2
            S.op("pe", lambda e, cur=cur: e.matmul(po[0][:, 0:1], U[:], cur[:, F - 1:F], start=True, stop=True), reads=[ck, "U"], writes=["po0"])
            S.op("dve", lambda e: e.tensor_scalar(out=off[:], in0=po[0][:, 0:1], scalar1=-1.0, scalar2=None, op0=ALU.add), reads=["po0"], writes=["off"])
            S.op("dve", lambda e, cur=cur: e.tensor_scalar(out=dest[:], in0=cur[:], scalar1=off[:, 0:1], scalar2=-BIGIDX, op0=ALU.add, op1=ALU.add),
                 reads=[ck, "off"], writes=["dest"])
            S.op("dve", lambda e: e.tensor_tensor(out=dest[:], in0=dest[:], in1=m[:], op=ALU.mult), reads=["dest", "m"], writes=["dest"])
            S.op("dve", lambda e: e.tensor_scalar(out=dest[:], in0=dest[:], scalar1=BIGIDX, scalar2=None, op0=ALU.add), reads=["dest"], writes=["dest"])
            S.op("dve", lambda e: e.tensor_copy(out=desti[:], in_=dest[:]), reads=["dest"], writes=["desti"])
            for f in range(F):
                _ind_dma(S, nc, ["desti", "tid2", f"list0_{ex}"], [f"lists_{ex}_{f}"], out=lists[ex],
                         out_offset=bass.IndirectOffsetOnAxis(ap=desti[:, f:f + 1], axis=0), in_=tid2[:, f, :], in_offset=None,
                         bounds_check=reg_cap, oob_is_err=False)
            S.dma("sp", lst[ex][:], lists[ex].rearrange("(b p) o -> p b o", p=128), reads=[f"lists_{ex}_{f}" for f in range(F)], writes=[f"lst{ex}"])
        ci = di = gi = oi = xi = 0
        for ex in range(EPC):
            S.dma("sp", bd[:], bd_d[ex:ex + 1, :].to_broadcast([128, D]), writes=["bd"])
            for s in range(CAP // MT2):
                for blk in range(MT2 // 128):
                    gb = s * (MT2 // 128) + blk
                    x_ = xg[xi % 2]
                    xk = f"xg{xi % 2}"
                    xi += 1
                    _ind_dma(S, nc, [f"lst{ex}"], [xk], out=x_[:], out_offset=None, in_=h_d,
                             in_offset=bass.IndirectOffsetOnAxis(ap=lst[ex][:, gb, 0:1], axis=0))
                    _ind_dma(S, nc, [f"lst{ex}"], ["gsl"], out=gsl[:, blk, :], out_offset=None, in_=gtm_d,
                             in_offset=bass.IndirectOffsetOnAxis(ap=lst[ex][:, gb, 0:1], axis=0))
                    S.group("pe", [(lambda e, k=k, x_=x_: e.transpose(out=pT[:, k, :], in_=x_[:, k * 128:(k + 1) * 128], identity=identb[:]))
                                   for k in range(KC)], reads=[xk, "identb"], writes=["pT"])
                    S.op("act", lambda e, blk=blk: e.activation(out=XT[:, :, blk * 128:(blk + 1) * 128], in_=pT[:], func=ACTF.Copy),
                         reads=["pT"], writes=["XT"])
                for j in range(KC):
                    w = wgl[ci % 2]
                    wk = f"wgl{ci % 2}"
                    ci += 1
                    S.dma("pool", w[:, 0, :, :], wg_d[ex, j], writes=[wk])
                    S.dma("pool", w[:, 1, :, :], wl_d[ex, j], writes=[wk])
                    for tg in range(MT2 // 512):
                        b = gi % 2
                        gi += 1
                        S.group("pe", [(lambda e_, k=k, w=w, b=b, tg=tg: e_.matmul(pg[b][:], w[:, 0, k, :], XT[:, k, tg * 512:(tg + 1) * 512],
                                                                                   start=(k == 0), stop=(k == KC - 1))) for k in range(KC)],
                                reads=[wk, "XT"], writes=[f"pg{b}"])
                        S.group("pe", [(lambda e_, k=k, w=w, b=b, tg=tg: e_.matmul(pl[b][:], w[:, 1, k, :], XT[:, k, tg * 512:(tg + 1) * 512],
                                                                                   start=(k == 0), stop=(k == KC - 1))) for k in range(KC)],
                                reads=[wk, "XT"], writes=[f"pl{b}"])
                        S.op("dve", lambda e_, b=b, ex=ex, j=j: e_.tensor_scalar(out=glu[b][:], in0=pg[b][:], scalar1=bgl[:, ex, 0, j:j + 1], scalar2=LIMIT,
                                                                            op0=ALU.add, op1=ALU.min), reads=[f"pg{b}", "bgl"], writes=[f"glu{b}"])
                        S.op("act", lambda e_, b=b: e_.activation(out=sig[b][:], in_=glu[b][:], func=ACTF.Sigmoid, scale=ALPHA),
                             reads=[f"glu{b}"], writes=[f"sig{b}"])
                        S.op("dve", lambda e_, b=b, ex=ex, j=j: e_.tensor_scalar(out=lin[b][:], in0=pl[b][:], scalar1=bgl[:, ex, 1, j:j + 1], scalar2=LIMIT,
                                                                            op0=ALU.add, op1=ALU.min), reads=[f"pl{b}", "bgl"], writes=[f"lin{b}"])
                        S.op("dve", lambda e_, b=b: e_.tensor_scalar(out=lin[b][:], in0=lin[b][:], scalar1=-LIMIT, scalar2=1.0,
                                                                     op0=ALU.max, op1=ALU.add), reads=[f"lin{b}"], writes=[f"lin{b}"])
                        S.op("dve", lambda e_, b=b: e_.tensor_tensor(out=glu[b][:], in0=glu[b][:], in1=sig[b][:], op=ALU.mult),
                             reads=[f"glu{b}", f"sig{b}"], writes=[f"glu{b}"])
                        S.op("dve", lambda e_, b=b, j=j, tg=tg: e_.tensor_tensor(out=actT[:, j, tg * 512:(tg + 1) * 512], in0=glu[b][:], in1=lin[b][:], op=ALU.mult),
                             reads=[f"glu{b}", f"lin{b}"], writes=["actT"])
                wd_next = None
                for dg in range(D // DW):
                    if wd_next is None:
                        wd, dk = wdb[di % 2], f"wdb{di % 2}"
                        di += 1
                        S.dma("pool", wd[:], wd_d[ex, dg], writes=[dk])
                    else:
                        wd, dk = wd_next
                    if dg < D // DW - 1:
                        wd_next = (wdb[di % 2], f"wdb{di % 2}")
                        di += 1
                        S.dma("pool", wd_next[0][:], wd_d[ex, dg + 1], writes=[wd_next[1]])
                    for t in range(MT2 // 128):
                        b = oi % 2
                        tb = oi % 4
                        oi += 1
                        gb = s * (MT2 // 128) + t
                        S.group("pe", [(lambda e_, k=k, wd=wd, b=b, t=t: e_.matmul(po[b][:, 0:DW], actT[:, k, t * 128:(t + 1) * 128], wd[:, k, :],
                                                                                   start=(k == 0), stop=(k == KC - 1))) for k in range(KC)],
                                reads=["actT", dk], writes=[f"po{b}"])
                        S.op("dve", lambda e_, b=b, tb=tb, dg=dg: e_.tensor_tensor(out=tmp[tb][:], in0=po[b][:, 0:DW], in1=bd[:, dg * DW:(dg + 1) * DW], op=ALU.add),
                             reads=[f"po{b}", "bd"], writes=[f"tmp{tb}"])
                        S.op("dve", lambda e_, tb=tb, t=t, ex=ex: e_.tensor_scalar(out=tmp[tb][:], in0=tmp[tb][:], scalar1=gsl[:, t, ex:ex + 1], scalar2=None, op0=ALU.mult),
                             reads=[f"tmp{tb}", "gsl"], writes=[f"tmp{tb}"])
                        _ind_dma(S, nc, [f"tmp{tb}", f"lst{ex}", "y0", f"y_{ex - 1}"], [f"y_{ex}"], out=y_o[dg],
                                 out_offset=bass.IndirectOffsetOnAxis(ap=lst[ex][:, gb, 0:1], axis=0), in_=tmp[tb][:], in_offset=None,
                                 compute_op=ALU.add, oob_is_err=True, bounds_check=reg_row)
        S.finish()
    return nc


MT3 = 1024


def build_moe3(NT):
    F = NT // 128
    NB = CAP // 128
    NROW = NT + 128
    nc = _new_nc()
    din = lambda n, sh, dt=F32: nc.dram_tensor(n, sh, dt, kind="ExternalInput").ap()
    h_d = din("htm", [NROW, D], BF16)
    gpm_d = din("gpm", [128, EPC, F])
    U_d = din("U", [128, 128])
    tid_d = din("tid", [128, F])
    pid_d = din("pid", [128, 1])
    identb_d = din("identb", [128, 128], BF16)
    wg_d = din("wg", [EPC, KC, 128, KC, 128])
    wl_d = din("wl", [EPC, KC, 128, KC, 128])
    bgl_d = din("bgl", [128, EPC, 2, KC])
    wd_d = din("wdf", [EPC, 128, KC, D])
    bd_d = din("bd", [EPC, D])
    y_o = nc.dram_tensor("ypart", [NROW, D], F32, kind="ExternalOutput").ap()
    lists = [nc.dram_tensor(f"lists{ex}", [CAP, 2], I32).ap() for ex in range(EPC)]
    with contextlib.ExitStack() as st:
        S = Sched(nc, st)
        sb = lambda n, sh, dt: st.enter_context(nc.sbuf_tensor(_uq("sb_" + n), sh, dt))
        ps = lambda n, sh, dt: st.enter_context(nc.psum_tensor(_uq("ps_" + n), sh, dt))
        XT = sb("XT", [128, KC, MT3], BF16)
        actT = sb("actT", [128, KC, MT3], BF16)
        wgl = [sb(f"wgl{i}", [128, 2, KC, 128], BF16) for i in range(2)]
        wdf = sb("wdf", [128, KC, D], BF16)
        yblk = [sb(f"yblk{i}", [128, D], F32) for i in range(2)]
        bd = sb("bd", [128, D], F32)
        bgl = sb("bgl", [128, EPC, 2, KC], F32)
        glu = [sb(f"glu{i}", [128, 512], F32) for i in range(2)]
        sig = [sb(f"sig{i}", [128, 512], F32) for i in range(2)]
        lin = [sb(f"lin{i}", [128, 512], F32) for i in range(2)]
        tmp = [sb(f"tmp{i}", [128, 512], F32) for i in range(2)]
        xg = [sb(f"xg{i}", [128, D], BF16) for i in range(2)]
        identb = sb("identb", [128, 128], BF16)
        gpm = sb("gpm", [128, EPC, F], F32)
        U = sb("U", [128, 128], F32)
        tid = sb("tid", [128, F], F32)
        tid2 = sb("tid2", [128, F, 2], I32)
        pid = sb("pid", [128, 1], F32)
        m = sb("m", [128, F], F32)
        ca = sb("ca", [128, F], F32)
        cb = sb("cb", [128, F], F32)
        off = sb("off", [128, 1], F32)
        dest = sb("dest", [128, F], F32)
        desti = sb("desti", [128, F], I32)
        pref = sb("pref", [128, 1], F32)
        pre = sb("pre", [128, NB, 2], I32)
        lst = [sb(f"lst{e}", [128, NB, 2], I32) for e in range(EPC)]
        pT = ps("pT", [128, KC, 128], BF16)
        pg = [ps(f"pg{i}", [128, 512], F32) for i in range(2)]
        pl = [ps(f"pl{i}", [128, 512], F32) for i in range(2)]
        po = [ps(f"po{i}", [128, 512], F32) for i in range(2)]
        for n, t_, d_ in (("gpm", gpm, gpm_d), ("U", U, U_d), ("tid", tid, tid_d), ("pid", pid, pid_d), ("identb", identb, identb_d), ("bgl", bgl, bgl_d)):
            S.dma("sp", t_[:], d_, writes=[n])
        reg_cap = nc.gpsimd.alloc_register("bc_cap")
        nc.gpsimd.reg_mov(reg_cap, CAP - 1)
        reg_row = nc.gpsimd.alloc_register("bc_row")
        nc.gpsimd.reg_mov(reg_row, NROW - 1)
        S.op("dve", lambda e: e.memset(bd[:], 0.0), writes=["bd"])
        for r in range(0, NROW, 1024):
            n = min(1024, NROW - r)
            S.dma("sp", y_o[r:r + n, :].rearrange("(a p) d -> p a d", p=128), bd[:].unsqueeze(1).to_broadcast([128, n // 128, D]),
                  reads=["bd"], writes=["y0"])
        S.op("dve", lambda e: e.tensor_scalar(out=pref[:], in0=pid[:], scalar1=float(NT), scalar2=None, op0=ALU.add), reads=["pid"], writes=["pref"])
        S.op("dve", lambda e: e.memset(pre[:], 0), writes=["pre"])
        S.op("dve", lambda e: e.tensor_copy(out=pre[:, :, 0], in_=pref[:, 0:1].to_broadcast([128, NB])), reads=["pref", "pre"], writes=["pre"])
        S.op("dve", lambda e: e.tensor_copy(out=tid2[:, :, 0], in_=tid[:]), reads=["tid"], writes=["tid2"])
        for ex in range(EPC):
            S.dma("sp", lists[ex].rearrange("(b p) o -> p b o", p=128), pre[:], reads=["pre"], writes=[f"list0_{ex}"])
            S.op("dve", lambda e, ex=ex: e.tensor_copy(out=tid2[:, :, 1], in_=gpm[:, ex, :].bitcast(I32)), reads=["gpm", "tid2"], writes=["tid2"])
            S.op("dve", lambda e, ex=ex: e.tensor_scalar(out=m[:], in0=gpm[:, ex, :], scalar1=0.0, scalar2=None, op0=ALU.is_gt), reads=["gpm"], writes=["m"])
            S.op("dve", lambda e: e.tensor_copy(out=ca[:], in_=m[:]), reads=["m"], writes=["ca"])
            cur, nxt, ck, nk = ca, cb, "ca", "cb"
            s_ = 1
            while s_ < F:
                S.op("dve", lambda e, cur=cur, nxt=nxt, s_=s_: e.tensor_copy(out=nxt[:, 0:s_], in_=cur[:, 0:s_]), reads=[ck], writes=[nk])
                S.op("dve", lambda e, cur=cur, nxt=nxt, s_=s_: e.tensor_tensor(out=nxt[:, s_:F], in0=cur[:, s_:F], in1=cur[:, 0:F - s_], op=ALU.add),
                     reads=[ck], writes=[nk])
                cur, nxt, ck, nk = nxt, cur, nk, ck
                s_ *= 2
            S.op("pe", lambda e, cur=cur: e.matmul(po[0][:, 0:1], U[:], cur[:, F - 1:F], start=True, stop=True), reads=[ck, "U"], writes=["po0"])
            S.op("dve", lambda e: e.tensor_scalar(out=off[:], in0=po[0][:, 0:1], scalar1=-1.0, scalar2=None, op0=ALU.add), reads=["po0"], writes=["off"])
            S.op("dve", lambda e, cur=cur: e.tensor_scalar(out=dest[:], in0=cur[:], scalar1=off[:, 0:1], scalar2=-BIGIDX, op0=ALU.add, op1=ALU.add),
                 reads=[ck, "off"], writes=["dest"])
            S.op("dve", lambda e: e.tensor_tensor(out=dest[:], in0=dest[:], in1=m[:], op=ALU.mult), reads=["dest", "m"], writes=["dest"])
            S.op("dve", lambda e: e.tensor_scalar(out=dest[:], in0=dest[:], scalar1=BIGIDX, scalar2=None, op0=ALU.add), reads=["dest"], writes=["dest"])
            S.op("dve", lambda e: e.tensor_copy(out=desti[:], in_=dest[:]), reads=["dest"], writes=["desti"])
            for f in range(F):
                _ind_dma(S, nc, ["desti", "tid2", f"list0_{ex}"], [f"lists_{ex}_{f}"], out=lists[ex],
                         out_offset=bass.IndirectOffsetOnAxis(ap=desti[:, f:f + 1], axis=0), in_=tid2[:, f, :], in_offset=None,
                         bounds_check=reg_cap, oob_is_err=False)
            S.dma("sp", lst[ex][:], lists[ex].rearrange("(b p) o -> p b o", p=128), reads=[f"lists_{ex}_{f}" for f in range(F)], writes=[f"lst{ex}"])
        ci = di = gi = oi = xi = yi = 0
        for ex in range(EPC):
            S.dma("sp", bd[:], bd_d[ex:ex + 1, :].to_broadcast([128, D]), writes=["bd"])
            for c4 in range(4):
                S.dma("pool", wdf[:, :, c4 * 512:(c4 + 1) * 512], wd_d[ex, :, :, c4 * 512:(c4 + 1) * 512], writes=["wdf"])
            for s in range(CAP // MT3):
                for blk in range(MT3 // 128):
                    gb = s * (MT3 // 128) + blk
                    x_ = xg[xi % 2]
                    xk = f"xg{xi % 2}"
                    xi += 1
                    _ind_dma(S, nc, [f"lst{ex}"], [xk], out=x_[:], out_offset=None, in_=h_d,
                             in_offset=bass.IndirectOffsetOnAxis(ap=lst[ex][:, gb, 0:1], axis=0))
                    S.group("pe", [(lambda e, k=k, x_=x_: e.transpose(out=pT[:, k, :], in_=x_[:, k * 128:(k + 1) * 128], identity=identb[:]))
                                   for k in range(KC)], reads=[xk, "identb"], writes=["pT"])
                    S.op("act", lambda e, blk=blk: e.activation(out=XT[:, :, blk * 128:(blk + 1) * 128], in_=pT[:], func=ACTF.Copy),
                         reads=["pT"], writes=["XT"])
                for j in range(KC):
                    w = wgl[ci % 2]
                    wk = f"wgl{ci % 2}"
                    ci += 1
                    S.dma("pool", w[:, 0, :, :], wg_d[ex, j], writes=[wk])
                    S.dma("pool", w[:, 1, :, :], wl_d[ex, j], writes=[wk])
                    for tg in range(MT3 // 512):
                        b = gi % 2
                        gi += 1
                        S.group("pe", [(lambda e_, k=k, w=w, b=b, tg=tg: e_.matmul(pg[b][:], w[:, 0, k, :], XT[:, k, tg * 512:(tg + 1) * 512],
                                                                                   start=(k == 0), stop=(k == KC - 1))) for k in range(KC)],
                                reads=[wk, "XT"], writes=[f"pg{b}"])
                        S.group("pe", [(lambda e_, k=k, w=w, b=b, tg=tg: e_.matmul(pl[b][:], w[:, 1, k, :], XT[:, k, tg * 512:(tg + 1) * 512],
                                                                                   start=(k == 0), stop=(k == KC - 1))) for k in range(KC)],
                                reads=[wk, "XT"], writes=[f"pl{b}"])
                        S.op("dve", lambda e_, b=b, ex=ex, j=j: e_.tensor_scalar(out=glu[b][:], in0=pg[b][:], scalar1=bgl[:, ex, 0, j:j + 1], scalar2=LIMIT,
                                                                            op0=ALU.add, op1=ALU.min), reads=[f"pg{b}", "bgl"], writes=[f"glu{b}"])
                        S.op("act", lambda e_, b=b: e_.activation(out=sig[b][:], in_=glu[b][:], func=ACTF.Sigmoid, scale=ALPHA),
                             reads=[f"glu{b}"], writes=[f"sig{b}"])
                        S.op("dve", lambda e_, b=b, ex=ex, j=j: e_.tensor_scalar(out=lin[b][:], in0=pl[b][:], scalar1=bgl[:, ex, 1, j:j + 1], scalar2=LIMIT,
                                                                            op0=ALU.add, op1=ALU.min), reads=[f"pl{b}", "bgl"], writes=[f"lin{b}"])
                        S.op("dve", lambda e_, b=b: e_.tensor_scalar(out=lin[b][:], in0=lin[b][:], scalar1=-LIMIT, scalar2=1.0,
                                                                     op0=ALU.max, op1=ALU.add), reads=[f"lin{b}"], writes=[f"lin{b}"])
                        S.op("dve", lambda e_, b=b: e_.tensor_tensor(out=glu[b][:], in0=glu[b][:], in1=sig[b][:], op=ALU.mult),
                             reads=[f"glu{b}", f"sig{b}"], writes=[f"glu{b}"])
                        S.op("dve", lambda e_, b=b, j=j, tg=tg: e_.tensor_tensor(out=actT[:, j, tg * 512:(tg + 1) * 512], in0=glu[b][:], in1=lin[b][:], op=ALU.mult),
                             reads=[f"glu{b}", f"lin{b}"], writes=["actT"])
                for t in range(MT3 // 128):
                    gb = s * (MT3 // 128) + t
                    yb = yblk[yi % 2]
                    yk = f"yblk{yi % 2}"
                    yi += 1
                    gcol = lst[ex][:, gb, 1:2].bitcast(F32)
                    for dg in range(4):
                        b = oi % 2
                        oi += 1
                        S.group("pe", [(lambda e_, k=k, b=b, t=t, dg=dg: e_.matmul(po[b][:], actT[:, k, t * 128:(t + 1) * 128], wdf[:, k, dg * 512:(dg + 1) * 512],
                                                                                   start=(k == 0), stop=(k == KC - 1))) for k in range(KC)],
                                reads=["actT", "wdf"], writes=[f"po{b}"])
                        S.op("dve", lambda e_, b=b, dg=dg: e_.tensor_tensor(out=tmp[b][:], in0=po[b][:], in1=bd[:, dg * 512:(dg + 1) * 512], op=ALU.add),
                             reads=[f"po{b}", "bd"], writes=[f"tmp{b}"])
                        S.op("dve", lambda e_, b=b, dg=dg, yb=yb, gcol=gcol: e_.tensor_scalar(out=yb[:, dg * 512:(dg + 1) * 512], in0=tmp[b][:], scalar1=gcol, scalar2=None, op0=ALU.mult),
                             reads=[f"tmp{b}", f"lst{ex}"], writes=[yk])
                    _ind_dma(S, nc, [yk, f"lst{ex}", "y0", f"y_{ex - 1}"], [f"y_{ex}"], out=y_o,
                             out_offset=bass.IndirectOffsetOnAxis(ap=lst[ex][:, gb, 0:1], axis=0), in_=yb[:], in_offset=None,
                             compute_op=ALU.add, oob_is_err=True, bounds_check=reg_row)
        S.finish()
    return nc


def moe2_inputs(core, htm_all, gates_all, NT):
    F = NT // 128
    g = gates_all[:, core * EPC:(core + 1) * EPC]
    gpm = np.ascontiguousarray(g.reshape(128, F, EPC).transpose(0, 2, 1))
    tid = (np.arange(128)[:, None] * F + np.arange(F)[None, :]).astype(np.float32)
    return {"htm": htm_all, "gpm": gpm, "U": np.triu(np.ones((128, 128), np.float32), 1),
            "tid": tid, "pid": np.arange(128, dtype=np.float32)[:, None],
            "identb": np.eye(128, dtype=np.float32).astype(ml_dtypes.bfloat16)}


def _run_moe2(NT, htm_list, gates_list, w_gu, b_gu, w_down, b_down):
    htm_all = np.ascontiguousarray(np.concatenate(htm_list + [np.zeros((128, D), ml_dtypes.bfloat16)], axis=0))
    gates_all = np.concatenate(gates_list, axis=0)
    ims = []
    for c in range(NCORE):
        m = moe_weights(c, w_gu, b_gu, w_down, b_down)
        m.update(moe2_inputs(c, htm_all, gates_all, NT))
        ims.append(m)
    res = _run(build_moe3(NT), ims)
    return [np.asarray(r["ypart"])[:NT] for r in res]
```
